# Optimizing a Trainium2 kernel written in Bass

```python
import math
import jax
import jax.numpy as jnp
from jax import lax
import numpy as np

D_MODEL = 2048
BATCH = 4
SEQ = 2048
DEPTH = 2

HEAD_DIM = 128
GDN_HEADS = 8
GDN_CONV = 4
GDN_CHUNK = 64
GDN_WIDTH = GDN_HEADS * HEAD_DIM
HGRN_HEADS = 8
HGRN_CHUNK = 16
HGRN_WIDTH = HGRN_HEADS * HEAD_DIM
DIL_GROUPS = ((128, 1), (512, 4), (2048, 16))
DIL_HEADS_PER_GROUP = 4
DIL_HEADS = len(DIL_GROUPS) * DIL_HEADS_PER_GROUP
DIL_WIDTH = DIL_HEADS * HEAD_DIM
MOBA_HEADS = 4
MOBA_BLOCK = 256
MOBA_TOPK = 3
MOBA_QUERY_CHUNK = 32
MOBA_WIDTH = MOBA_HEADS * HEAD_DIM
ROPE_THETA = 10000.0
N_EXPERTS = 16
N_EXPERT_GROUPS = 4
EXPERTS_PER_GROUP = N_EXPERTS // N_EXPERT_GROUPS
TOP_K = 2
D_EXPERT = 512
DEEPNORM_ALPHA = (2.0 * DEPTH) ** 0.25
DEEPNORM_BETA = (8.0 * DEPTH) ** -0.25
LN_EPS = 1e-5
RMS_EPS = 1e-6
NEG_INF = -1e30

_EVEN_PARTS = (3 * GDN_WIDTH, GDN_WIDTH, GDN_HEADS, GDN_HEADS, HGRN_WIDTH, HGRN_WIDTH, HGRN_WIDTH, HGRN_WIDTH)
EVEN_IN = sum(_EVEN_PARTS)
EVEN_SPLITS = tuple(int(s) for s in np.cumsum(_EVEN_PARTS)[:-1])
EVEN_OUT = GDN_WIDTH + HGRN_WIDTH
_ODD_PARTS = (DIL_WIDTH, DIL_WIDTH, DIL_WIDTH, MOBA_WIDTH, MOBA_WIDTH, MOBA_WIDTH)
ODD_IN = sum(_ODD_PARTS)
ODD_SPLITS = tuple(int(s) for s in np.cumsum(_ODD_PARTS)[:-1])
ODD_OUT = (DIL_HEADS_PER_GROUP + MOBA_HEADS) * HEAD_DIM
N_EVEN = (DEPTH + 1) // 2
N_ODD = DEPTH // 2

kernel_name = 'hybrid_gdn_hgrn2_dilated_moba_grouped_moe'


def layer_norm(x, gain, bias):
    xf = x.astype(jnp.float32)
    mu = jnp.mean(xf, axis=-1, keepdims=True)
    var = jnp.mean(jnp.square(xf - mu), axis=-1, keepdims=True)
    y = (xf - mu) * lax.rsqrt(var + LN_EPS) * gain.astype(jnp.float32) + bias.astype(jnp.float32)
    return y.astype(x.dtype)


def rms_norm(x, gain):
    return x * lax.rsqrt(jnp.mean(jnp.square(x), axis=-1, keepdims=True) + RMS_EPS) * gain.astype(jnp.float32)


def l2_normalize(x):
    return x * lax.rsqrt(jnp.sum(jnp.square(x), axis=-1, keepdims=True) + RMS_EPS)


def split_heads(x, n_heads):
    b, s, _ = x.shape
    return x.reshape(b, s, n_heads, -1).transpose(0, 2, 1, 3)


def merge_heads(x):
    b, n, s, dh = x.shape
    return x.transpose(0, 2, 1, 3).reshape(b, s, n * dh)


def to_chunks(x, c):
    b, h, s = x.shape[:3]
    return jnp.moveaxis(x.reshape(b, h, s // c, c, *x.shape[3:]), 2, 0)


def from_chunks(x):
    n, b, h, c, d = x.shape
    return jnp.moveaxis(x, 0, 2).reshape(b, h, n * c, d)


def rope_tables(seq_len, dim):
    inv_freq = ROPE_THETA ** (-jnp.arange(0, dim, 2, dtype=jnp.float32) / dim)
    ang = jnp.arange(seq_len, dtype=jnp.float32)[:, None] * inv_freq[None, :]
    return jnp.cos(ang), jnp.sin(ang)


def apply_rope(x, cos, sin):
    x1, x2 = jnp.split(x, 2, axis=-1)
    return jnp.concatenate([x1 * cos - x2 * sin, x2 * cos + x1 * sin], axis=-1)


def causal_short_conv(x, w):
    k = w.shape[0]
    s = x.shape[1]
    xp = jnp.pad(x, ((0, 0), (k - 1, 0), (0, 0)))
    return sum(xp[:, j:j + s, :] * w[j] for j in range(k))


def gated_delta_rule_chunked(q, k, v, g, beta):
    c = GDN_CHUNK
    b, h, s, dk = q.shape
    dv = v.shape[-1]
    qc, kc, vc = to_chunks(q, c), to_chunks(k, c), to_chunks(v, c)
    gcum = jnp.cumsum(to_chunks(g, c), axis=-1)
    bc = to_chunks(beta, c)
    strict = jnp.tril(jnp.ones((c, c), bool), -1)
    incl = jnp.tril(jnp.ones((c, c), bool))
    diff = gcum[..., :, None] - gcum[..., None, :]
    dec_strict = jnp.where(strict, jnp.exp(jnp.where(strict, diff, 0.0)), 0.0)
    dec_incl = jnp.where(incl, jnp.exp(jnp.where(incl, diff, 0.0)), 0.0)
    kk = jnp.einsum('nbhid,nbhjd->nbhij', kc, kc)
    a_mat = jnp.eye(c, dtype=q.dtype) + bc[..., :, None] * dec_strict * kk
    rhs = jnp.concatenate([bc[..., None] * vc, (bc * jnp.exp(gcum))[..., None] * kc], axis=-1)
    sol = lax.linalg.triangular_solve(a_mat, rhs, left_side=True, lower=True, unit_diagonal=True)
    u_base, w_mat = sol[..., :dv], sol[..., dv:]
    qk = jnp.einsum('nbhid,nbhjd->nbhij', qc, kc) * dec_incl
    q_dec = qc * jnp.exp(gcum)[..., None]
    k_dec = kc * jnp.exp(gcum[..., -1:] - gcum)[..., None]
    chunk_dec = jnp.exp(gcum[..., -1])

    def step(state, inp):
        u_b, w_c, q_d, qk_c, k_d, d_c = inp
        u = u_b - jnp.einsum('bhck,bhkv->bhcv', w_c, state)
        o = jnp.einsum('bhck,bhkv->bhcv', q_d, state) + jnp.einsum('bhij,bhjv->bhiv', qk_c, u)
        state = d_c[..., None, None] * state + jnp.einsum('bhck,bhcv->bhkv', k_d, u)
        return state, o

    s0 = jnp.zeros((b, h, dk, dv), q.dtype)
    _, o = lax.scan(step, s0, (u_base, w_mat, q_dec, qk, k_dec, chunk_dec))
    return from_chunks(o)


def hgrn2_chunked(q, f, i):
    c = HGRN_CHUNK
    b, h, s, fd = q.shape
    vd = i.shape[-1]
    qc, ic, fc = to_chunks(q, c), to_chunks(i, c), to_chunks(f, c)
    kc = 1.0 - fc
    bcum = jnp.cumsum(jnp.log(fc), axis=-2)
    q_dec = qc * jnp.exp(bcum)
    k_inv = kc * jnp.exp(-bcum)
    k_dec = kc * jnp.exp(bcum[..., -1:, :] - bcum)
    incl = jnp.tril(jnp.ones((c, c), bool))
    p = jnp.where(incl, jnp.einsum('nbhif,nbhjf->nbhij', q_dec, k_inv), 0.0)
    o_intra = jnp.einsum('nbhij,nbhjv->nbhiv', p, ic)
    chunk_dec = jnp.exp(bcum[..., -1, :])

    def step(state, inp):
        q_d, k_d, i_c, d_c = inp
        o = jnp.einsum('bhcf,bhfv->bhcv', q_d, state)
        state = d_c[..., None] * state + jnp.einsum('bhcf,bhcv->bhfv', k_d, i_c)
        return state, o

    s0 = jnp.zeros((b, h, fd, vd), q.dtype)
    _, o_inter = lax.scan(step, s0, (q_dec, k_dec, ic, chunk_dec))
    return from_chunks(o_intra + o_inter)


def even_mixer(x, w_in, conv_w, a_log, dt_bias, gdn_norm, hgrn_norm, lower_bound, w_out):
    f32 = jnp.float32
    h = jnp.einsum('bsd,de->bse', x, w_in).astype(f32)
    qkv_a, z_a, b_a, a_a, q_b, f_b, i_b, g_b = jnp.split(h, EVEN_SPLITS, axis=-1)
    qkv_a = jax.nn.silu(causal_short_conv(qkv_a, conv_w.astype(f32)))
    q_a, k_a, v_a = jnp.split(qkv_a, 3, axis=-1)
    q_a = l2_normalize(split_heads(q_a, GDN_HEADS)) * HEAD_DIM ** -0.5
    k_a = l2_normalize(split_heads(k_a, GDN_HEADS))
    v_a = split_heads(v_a, GDN_HEADS)
    beta = jax.nn.sigmoid(b_a).transpose(0, 2, 1)
    g = (-jnp.exp(a_log.astype(f32)) * jax.nn.softplus(a_a + dt_bias.astype(f32))).transpose(0, 2, 1)
    o_a = gated_delta_rule_chunked(q_a, k_a, v_a, g, beta)
    o_a = rms_norm(o_a, gdn_norm) * jax.nn.silu(split_heads(z_a, GDN_HEADS))
    lb = lower_bound.astype(f32).reshape(HGRN_HEADS, 1, HEAD_DIM)
    f = lb + (1.0 - lb) * jax.nn.sigmoid(split_heads(f_b, HGRN_HEADS))
    o_b = hgrn2_chunked(split_heads(q_b, HGRN_HEADS), f, split_heads(i_b, HGRN_HEADS))
    o_b = rms_norm(o_b, hgrn_norm) * jax.nn.silu(split_heads(g_b, HGRN_HEADS))
    o = merge_heads(jnp.concatenate([o_a, o_b], axis=1))
    return jnp.einsum('bse,ed->bsd', o.astype(x.dtype), w_out)


def banded_causal_attention(q, k, v, window):
    *lead, length, dh = q.shape
    w = window
    nb = -(-length // w)
    lp = nb * w
    lead_pad = [(0, 0)] * len(lead)
    qb = jnp.pad(q, lead_pad + [(0, lp - length), (0, 0)]).reshape(*lead, nb, w, dh)
    kp = jnp.pad(k, lead_pad + [(w, lp - length), (0, 0)]).reshape(*lead, nb + 1, w, dh)
    vp = jnp.pad(v, lead_pad + [(w, lp - length), (0, 0)]).reshape(*lead, nb + 1, w, dh)
    kcat = jnp.concatenate([kp[..., :-1, :, :], kp[..., 1:, :, :]], axis=-2)
    vcat = jnp.concatenate([vp[..., :-1, :, :], vp[..., 1:, :, :]], axis=-2)
    s = jnp.einsum('...nqd,...nkd->...nqk', qb, kcat) * dh ** -0.5
    r = jnp.arange(w)[:, None]
    c = jnp.arange(2 * w)[None, :]
    dist = r + w - c
    kpos = (jnp.arange(nb) * w)[:, None, None] + c[None] - w
    mask = (dist >= 0)[None] & (dist <= w)[None] & (kpos >= 0)
    s = jnp.where(mask, s, NEG_INF)
    m = jnp.max(s, axis=-1, keepdims=True)
    p = jnp.exp(s - m)
    den = jnp.sum(p, axis=-1)
    o = jnp.einsum('...nqk,...nkd->...nqd', p, vcat) / den[..., None]
    lse = m[..., 0] + jnp.log(den)
    o = o.reshape(*lead, lp, dh)[..., :length, :]
    lse = lse.reshape(*lead, lp)[..., :length]
    return o, lse


def dilated_window_attention(q, k, v, window, dilation):
    b, h, s, dh = q.shape
    length = s // dilation

    def to_sub(t):
        return t.reshape(b, h, length, dilation, dh).transpose(0, 1, 3, 2, 4)

    o, lse = banded_causal_attention(to_sub(q), to_sub(k), to_sub(v), window // dilation)
    o = o.transpose(0, 1, 3, 2, 4).reshape(b, h, s, dh)
    lse = lse.transpose(0, 1, 3, 2).reshape(b, h, s)
    return o, lse


def moba_attention(q, k, v):
    b, h, s, dh = q.shape
    blk = MOBA_BLOCK
    sp = -(-s // blk) * blk
    pad = ((0, 0), (0, 0), (0, sp - s), (0, 0))
    q, k, v = (jnp.pad(t, pad) for t in (q, k, v))
    n_blk = sp // blk
    k_blocks = k.reshape(b, h, n_blk, blk, dh)
    v_blocks = v.reshape(b, h, n_blk, blk, dh)
    k_mean = jnp.mean(k_blocks, axis=3)
    q_block_id = jnp.arange(sp) // blk
    fully_past = jnp.arange(n_blk)[None, :] < q_block_id[:, None]
    gate = jnp.where(fully_past, jnp.einsum('bhsd,bhnd->bhsn', q, k_mean), NEG_INF)
    top_k = min(MOBA_TOPK, n_blk)
    _, sel = lax.top_k(gate, top_k)
    sel_valid = jnp.arange(top_k)[None, :] < q_block_id[:, None]
    scale = dh ** -0.5
    qn = MOBA_QUERY_CHUNK
    b_idx = jnp.arange(b)[:, None, None, None]
    h_idx = jnp.arange(h)[None, :, None, None]

    def attend_chunk(ci):
        start = ci * qn
        q_c = lax.dynamic_slice_in_dim(q, start, qn, axis=2)
        sel_c = lax.dynamic_slice_in_dim(sel, start, qn, axis=2)
        valid_c = lax.dynamic_slice_in_dim(sel_valid, start, qn, axis=0)
        k_sel = k_blocks[b_idx, h_idx, sel_c]
        v_sel = v_blocks[b_idx, h_idx, sel_c]
        s_sel = jnp.einsum('bhqd,bhqnjd->bhqnj', q_c, k_sel) * scale
        s_sel = jnp.where(valid_c[None, None, :, :, None], s_sel, NEG_INF).reshape(b, h, qn, top_k * blk)
        own = start // blk
        k_own = lax.dynamic_index_in_dim(k_blocks, own, axis=2, keepdims=False)
        v_own = lax.dynamic_index_in_dim(v_blocks, own, axis=2, keepdims=False)
        q_pos = start + jnp.arange(qn)
        k_pos = own * blk + jnp.arange(blk)
        s_own = jnp.where(k_pos[None, :] <= q_pos[:, None],
                          jnp.einsum('bhqd,bhjd->bhqj', q_c, k_own) * scale, NEG_INF)
        p = jax.nn.softmax(jnp.concatenate([s_sel, s_own], axis=-1), axis=-1)
        p_sel = p[..., :top_k * blk].reshape(b, h, qn, top_k, blk)
        p_own = p[..., top_k * blk:]
        return jnp.einsum('bhqnj,bhqnjd->bhqd', p_sel, v_sel) + jnp.einsum('bhqj,bhjd->bhqd', p_own, v_own)

    out = lax.map(attend_chunk, jnp.arange(sp // qn))
    out = jnp.moveaxis(out, 0, 2).reshape(b, h, sp, dh)
    return out[:, :, :s]


def odd_mixer(x, w_in, w_out, cos, sin):
    h = jnp.einsum('bsd,de->bse', x, w_in).astype(jnp.float32)
    cq, ck, cv, dq, dk, dv = jnp.split(h, ODD_SPLITS, axis=-1)
    cq = apply_rope(split_heads(cq, DIL_HEADS), cos, sin)
    ck = apply_rope(split_heads(ck, DIL_HEADS), cos, sin)
    cv = split_heads(cv, DIL_HEADS)
    outs, lses = [], []
    for gi, (window, dilation) in enumerate(DIL_GROUPS):
        hs = slice(gi * DIL_HEADS_PER_GROUP, (gi + 1) * DIL_HEADS_PER_GROUP)
        o_g, l_g = dilated_window_attention(cq[:, hs], ck[:, hs], cv[:, hs], window, dilation)
        outs.append(o_g)
        lses.append(l_g)
    wts = jax.nn.softmax(jnp.stack(lses, axis=0), axis=0)
    o_c = jnp.sum(wts[..., None] * jnp.stack(outs, axis=0), axis=0)
    o_d = moba_attention(apply_rope(split_heads(dq, MOBA_HEADS), cos, sin),
                         apply_rope(split_heads(dk, MOBA_HEADS), cos, sin),
                         split_heads(dv, MOBA_HEADS))
    o = merge_heads(jnp.concatenate([o_c, o_d], axis=1))
    return jnp.einsum('bse,ed->bsd', o.astype(x.dtype), w_out)


def moe_ffn(x, router_w, router_bias, w_gate, w_up, w_down):
    b, s, d = x.shape
    t = x.reshape(b * s, d)
    scores = jax.nn.sigmoid(jnp.einsum('td,de->te', t, router_w).astype(jnp.float32))
    biased = scores + router_bias.astype(jnp.float32)
    grouped = biased.reshape(-1, N_EXPERT_GROUPS, EXPERTS_PER_GROUP)
    group_score = jnp.sum(lax.top_k(grouped, 2)[0], axis=-1)
    best_group = jnp.argmax(group_score, axis=-1)
    in_group = (jnp.arange(N_EXPERTS) // EXPERTS_PER_GROUP)[None, :] == best_group[:, None]
    _, top_idx = lax.top_k(jnp.where(in_group, biased, NEG_INF), TOP_K)
    top_scores = jnp.take_along_axis(scores, top_idx, axis=-1)
    weights = top_scores / jnp.sum(top_scores, axis=-1, keepdims=True)
    gates = jnp.sum(jax.nn.one_hot(top_idx, N_EXPERTS, dtype=jnp.float32) * weights[..., None], axis=1)
    hid = jax.nn.silu(jnp.einsum('td,edf->tef', t, w_gate)) * jnp.einsum('td,edf->tef', t, w_up)
    y = jnp.einsum('tef,efd->td', hid * gates[..., None].astype(hid.dtype), w_down)
    return y.reshape(b, s, d).astype(x.dtype)


def setup_inputs(seed: int = 0) -> dict:
    key = jax.random.key(seed)
    ks = jax.random.split(key, 20)
    f32 = jnp.float32

    def nrm(k, shape, scale):
        return jax.random.normal(k, shape, f32) * scale

    x = nrm(ks[0], (BATCH, SEQ, D_MODEL), 1.0)
    ev_w_in = nrm(ks[1], (N_EVEN, D_MODEL, EVEN_IN), D_MODEL ** -0.5)
    ev_conv_w = nrm(ks[2], (N_EVEN, GDN_CONV, 3 * GDN_WIDTH), GDN_CONV ** -0.5)
    ev_a_log = jnp.log(jax.random.uniform(ks[3], (N_EVEN, GDN_HEADS), f32, 1.0, 16.0))
    dt = jnp.exp(jax.random.uniform(ks[4], (N_EVEN, GDN_HEADS), f32, math.log(1e-3), math.log(1e-1)))
    ev_dt_bias = dt + jnp.log(-jnp.expm1(-dt))
    ev_gdn_norm = 1.0 + nrm(ks[5], (N_EVEN, HEAD_DIM), 0.02)
    ev_hgrn_norm = 1.0 + nrm(ks[6], (N_EVEN, HEAD_DIM), 0.02)
    hgrn_lb_logits = nrm(ks[7], (DEPTH + 1, HGRN_WIDTH), 0.1)
    ev_w_out = nrm(ks[8], (N_EVEN, EVEN_OUT, D_MODEL), EVEN_OUT ** -0.5 * DEEPNORM_BETA)
    od_w_in = nrm(ks[9], (N_ODD, D_MODEL, ODD_IN), D_MODEL ** -0.5)
    od_w_out = nrm(ks[10], (N_ODD, ODD_OUT, D_MODEL), ODD_OUT ** -0.5 * DEEPNORM_BETA)
    router_w = nrm(ks[11], (D_MODEL, N_EXPERTS), D_MODEL ** -0.5)
    router_bias = nrm(ks[12], (N_EXPERTS,), 0.01)
    moe_w_gate = nrm(ks[13], (DEPTH, N_EXPERTS, D_MODEL, D_EXPERT), D_MODEL ** -0.5)
    moe_w_up = nrm(ks[14], (DEPTH, N_EXPERTS, D_MODEL, D_EXPERT), D_MODEL ** -0.5)
    moe_w_down = nrm(ks[15], (DEPTH, N_EXPERTS, D_EXPERT, D_MODEL), D_EXPERT ** -0.5 * DEEPNORM_BETA)
    ln_gain = 1.0 + nrm(ks[16], (DEPTH, 2, D_MODEL), 0.02)
    ln_bias = nrm(ks[17], (DEPTH, 2, D_MODEL), 0.01)
    return {'x': x, 'ev_w_in': ev_w_in, 'ev_conv_w': ev_conv_w, 'ev_a_log': ev_a_log,
            'ev_dt_bias': ev_dt_bias, 'ev_gdn_norm': ev_gdn_norm, 'ev_hgrn_norm': ev_hgrn_norm,
            'hgrn_lb_logits': hgrn_lb_logits, 'ev_w_out': ev_w_out, 'od_w_in': od_w_in,
            'od_w_out': od_w_out, 'router_w': router_w, 'router_bias': router_bias,
            'moe_w_gate': moe_w_gate, 'moe_w_up': moe_w_up, 'moe_w_down': moe_w_down,
            'ln_gain': ln_gain, 'ln_bias': ln_bias}


def reference(x, ev_w_in, ev_conv_w, ev_a_log, ev_dt_bias, ev_gdn_norm, ev_hgrn_norm, hgrn_lb_logits,
              ev_w_out, od_w_in, od_w_out, router_w, router_bias, moe_w_gate, moe_w_up, moe_w_down,
              ln_gain, ln_bias):
    cos, sin = rope_tables(x.shape[1], HEAD_DIM)
    lower_bounds = jnp.cumsum(jax.nn.softmax(hgrn_lb_logits.astype(jnp.float32), axis=0), axis=0)
    h = x
    for layer in range(DEPTH):
        if layer % 2 == 0:
            e = layer // 2
            mix = even_mixer(h, ev_w_in[e], ev_conv_w[e], ev_a_log[e], ev_dt_bias[e], ev_gdn_norm[e],
                             ev_hgrn_norm[e], lower_bounds[layer], ev_w_out[e])
        else:
            o = layer // 2
            mix = odd_mixer(h, od_w_in[o], od_w_out[o], cos, sin)
        h = layer_norm(DEEPNORM_ALPHA * h + mix, ln_gain[layer, 0], ln_bias[layer, 0])
        ffn = moe_ffn(h, router_w, router_bias, moe_w_gate[layer], moe_w_up[layer], moe_w_down[layer])
        h = layer_norm(DEEPNORM_ALPHA * h + ffn, ln_gain[layer, 1], ln_bias[layer, 1])
    return h
```

```python
from contextlib import ExitStack
import numpy as np
import concourse.bass as bass
import concourse.mybir as mybir

F32 = mybir.dt.float32
BF16 = mybir.dt.bfloat16
I32 = mybir.dt.int32
U32 = mybir.dt.uint32
AF = mybir.ActivationFunctionType
ALU = mybir.AluOpType
AX = mybir.AxisListType

SAME_ENGINE_SYNC = True


class Buf:
    def __init__(self, t, name):
        self.t = t
        self.name = name
        self.w = None
        self.r = []

    def __getitem__(self, idx):
        return self.t[idx]


class Eng:
    def __init__(self, name, kind):
        self.name = name
        self.kind = kind
        self.items = []
        self.sem = None
        self.cnt = 0
        self.seen = {}
        self.dma_sems = []
        self.dma_cnts = []
        self.dma_rr = 0


class Prog:
    def __init__(self, n_dma_sems=4):
        self.nc = bass.Bass("TRN2", target_bir_lowering=False)
        self.es = ExitStack()
        self.eng = {}
        for name in ("pe", "act", "dve", "pool", "sp"):
            e = Eng(name, name)
            e.sem = self.es.enter_context(self.nc.semaphore("s_" + name))
            self.eng[name] = e
        for name in ("sp", "act", "pool"):
            e = self.eng[name]
            for k in range(n_dma_sems):
                e.dma_sems.append(self.es.enter_context(self.nc.semaphore("d_%s%d" % (name, k))))
                e.dma_cnts.append(0)
        self.nbuf = 0
        self.out_tokens = []

    def dram(self, name, shape, dtype, kind):
        return self.nc.dram_tensor(name, list(shape), dtype, kind=kind)

    def sbuf(self, name, shape, dtype):
        t = self.es.enter_context(self.nc.sbuf_tensor(name, list(shape), dtype))
        return Buf(t, name)

    def psum(self, name, shape, dtype=F32):
        t = self.es.enter_context(self.nc.psum_tensor(name, list(shape), dtype))
        return Buf(t, name)

    def track(self, t, name):
        return Buf(t, name)

    def _waits(self, e, reads, writes, own_sems):
        need = {}

        def add(tok):
            if tok is None:
                return
            sem, val = tok
            if (not SAME_ENGINE_SYNC or e.kind == "pe") and sem is e.sem and sem not in own_sems:
                return
            k = id(sem)
            if e.seen.get(k, 0) >= val:
                return
            if k not in need or need[k][1] < val:
                need[k] = (sem, val)

        for b in reads:
            add(b.w)
        for b in writes:
            add(b.w)
            for t in b.r:
                add(t)
        for k, (sem, val) in need.items():
            e.seen[k] = val
        return list(need.values())

    def op(self, engname, fn, reads=(), writes=()):
        e = self.eng[engname]
        ex = [b for b in reads if getattr(b, "excl", False)]
        if ex:
            reads = [b for b in reads if not getattr(b, "excl", False)]
            writes = list(writes) + ex
        waits = self._waits(e, reads, writes, ())
        e.cnt += 1
        tok = (e.sem, e.cnt)
        e.items.append((waits, fn, e.sem, 1))
        for b in reads:
            b.r.append(tok)
        for b in writes:
            b.w = tok
            b.r = []
        return tok

    def dma(self, engname, out, in_, reads=(), writes=(), is_output=False, **kw):
        e = self.eng[engname]
        waits = self._waits(e, reads, writes, ())
        k = e.dma_rr
        e.dma_rr = (e.dma_rr + 1) % len(e.dma_sems)
        e.dma_cnts[k] += 16
        sem = e.dma_sems[k]
        tok = (sem, e.dma_cnts[k])
        e.items.append((waits, lambda h: h.dma_start(out=out, in_=in_, **kw), sem, 16))
        for b in reads:
            b.r.append(tok)
        for b in writes:
            b.w = tok
            b.r = []
        if is_output:
            self.out_tokens.append(tok)
        return tok

    def finish(self):
        nc = self.nc
        sp = self.eng["sp"]
        final_waits = list(self.out_tokens)
        for name, e in self.eng.items():
            if name != "sp" and e.cnt:
                final_waits.append((e.sem, e.cnt))
        for e in self.eng.values():
            for s, c in zip(e.dma_sems, e.dma_cnts):
                if c:
                    final_waits.append((s, c))
        with nc.Block() as block:
            def emit(e, h, extra=()):
                for waits, fn, sem, inc in e.items:
                    for (s, v) in waits:
                        h.wait_ge(s, v)
                    fn(h).then_inc(sem, inc)
                for (s, v) in extra:
                    h.wait_ge(s, v)

            @block.sync
            def _(h):
                emit(self.eng["sp"], h, final_waits)

            @block.tensor
            def _(h):
                emit(self.eng["pe"], h)

            @block.scalar
            def _(h):
                emit(self.eng["act"], h)

            @block.vector
            def _(h):
                emit(self.eng["dve"], h)

            @block.gpsimd
            def _(h):
                emit(self.eng["pool"], h)
        self.es.close()
        return nc

    def n_instr(self):
        return {k: len(e.items) for k, e in self.eng.items()}

from concourse.bass_utils import run_bass_kernel_spmd

D = 2048
ALPHA = (2.0 * 2) ** 0.25
LN_EPS = 1e-5


def ln_tile(P, z, zb, scr, gain, bias, outb, tag, stats, mv, rstd):
    for i in range(4):
        P.op("dve", lambda h, i=i: h.bn_stats(out=stats[:, i * 6:(i + 1) * 6], in_=z[:, i * 512:(i + 1) * 512]),
             reads=[zb], writes=[stats])
    P.op("dve", lambda h: h.bn_aggr(out=mv[:], in_=stats[:]), reads=[stats], writes=[mv])
    P.op("dve", lambda h: h.tensor_scalar(out=rstd[:], in0=mv[:, 1:2], scalar1=LN_EPS, scalar2=None, op0=ALU.add),
         reads=[mv], writes=[rstd])
    P.op("act", lambda h: h.activation(out=rstd[:], in_=rstd[:], func=AF.Sqrt), reads=[rstd], writes=[rstd])
    P.op("dve", lambda h: h.reciprocal(out=rstd[:], in_=rstd[:]), reads=[rstd], writes=[rstd])
    P.op("dve", lambda h: h.tensor_scalar(out=scr[:], in0=z, scalar1=mv[:, 0:1], scalar2=rstd[:, 0:1],
                                          op0=ALU.subtract, op1=ALU.mult), reads=[zb, mv, rstd], writes=[scr])
    P.op("pool", lambda h: h.tensor_tensor(out=scr[:], in0=scr[:], in1=gain[:], op=ALU.mult), reads=[scr, gain], writes=[scr])
    P.op("pool", lambda h: h.tensor_tensor(out=outb[:], in0=scr[:], in1=bias[:], op=ALU.add), reads=[scr, bias], writes=[outb])


def build_k1(Ec):
    P = Prog()
    oT = P.dram("oT", [128, Ec, 1024], F32, "ExternalInput").ap()
    xres = P.dram("xres", [128, 8, D], F32, "ExternalInput").ap()
    w = P.dram("w", [128, Ec, D], F32, "ExternalInput").ap()
    lng = P.dram("lng", [128, D], F32, "ExternalInput").ap()
    lnb = P.dram("lnb", [128, D], F32, "ExternalInput").ap()
    hout = P.dram("hout", [128, 8, D], F32, "ExternalOutput").ap()
    oT_bf = P.sbuf("oT_bf", [128, Ec, 1024], BF16)
    w_bf = P.sbuf("w_bf", [128, Ec, D], BF16)
    st = [P.sbuf("st%d" % i, [128, D], F32) for i in range(2)]
    g_sb = P.sbuf("g_sb", [128, D], F32)
    b_sb = P.sbuf("b_sb", [128, D], F32)
    P.dma("pool", g_sb[:], lng, writes=[g_sb])
    P.dma("pool", b_sb[:], lnb, writes=[b_sb])
    k = 0
    for c in range(Ec):
        s = st[k % 2]; k += 1
        P.dma("sp", s[:, 0:1024], oT[:, c, :], writes=[s])
        P.op("act", lambda h, s=s, c=c: h.activation(out=oT_bf[:, c, :], in_=s[:, 0:1024], func=AF.Copy), reads=[s], writes=[oT_bf])
        s = st[k % 2]; k += 1
        P.dma("sp", s[:], w[:, c, :], writes=[s])
        P.op("dve", lambda h, s=s, c=c: h.tensor_copy(out=w_bf[:, c, :], in_=s[:]), reads=[s], writes=[w_bf])
    ps = [P.psum("ps%d" % i, [128, 512], F32) for i in range(4)]
    xr = [P.sbuf("xr%d" % i, [128, D], F32) for i in range(2)]
    z = [P.sbuf("z%d" % i, [128, D], F32) for i in range(2)]
    scr = P.sbuf("scr", [128, D], F32)
    ot = [P.sbuf("ot%d" % i, [128, D], F32) for i in range(2)]
    stats = P.sbuf("stats", [128, 24], F32)
    mv = P.sbuf("mv", [128, 2], F32)
    rstd = P.sbuf("rstd", [128, 1], F32)
    for n in range(8):
        xrb = xr[n % 2]; zb = z[n % 2]; ob = ot[n % 2]
        P.dma("sp", xrb[:], xres[:, n, :], writes=[xrb])
        for nb in range(4):
            pb = ps[nb]
            for c in range(Ec):
                P.op("pe", lambda h, pb=pb, c=c, n=n, nb=nb: h.matmul(pb[:], lhsT=oT_bf[:, c, n * 128:(n + 1) * 128],
                                                                 rhs=w_bf[:, c, nb * 512:(nb + 1) * 512],
                                                                 start=(c == 0), stop=(c == Ec - 1)),
                     reads=[oT_bf, w_bf], writes=[pb])
            P.op("dve", lambda h, pb=pb, nb=nb, xrb=xrb, zb=zb: h.scalar_tensor_tensor(
                out=zb[:, nb * 512:(nb + 1) * 512], in0=xrb[:, nb * 512:(nb + 1) * 512], scalar=ALPHA, in1=pb[:],
                op0=ALU.mult, op1=ALU.add), reads=[xrb, pb], writes=[zb])
        ln_tile(P, zb[:], zb, scr, g_sb, b_sb, ob, "k1", stats, mv, rstd)
        P.dma("sp", hout[:, n, :], ob[:], reads=[ob], is_output=True)
    return P.finish()


def build_k2():
    P = Prog()
    hT = P.dram("hT", [128, 16, 1024], F32, "ExternalInput").ap()
    htok = P.dram("htok", [128, 8, D], F32, "ExternalInput").ap()
    rw = P.dram("rw", [128, 16, 16], F32, "ExternalInput").ap()
    rb = P.dram("rb", [128, 16], F32, "ExternalInput").ap()
    wg = P.dram("wg", [16, D, 512], F32, "ExternalInput").ap()
    wu = P.dram("wu", [16, D, 512], F32, "ExternalInput").ap()
    wd = P.dram("wd", [16, 512, D], F32, "ExternalInput").ap()
    lng = P.dram("lng", [128, D], F32, "ExternalInput").ap()
    lnb = P.dram("lnb", [128, D], F32, "ExternalInput").ap()
    ident = P.dram("ident", [128, 128], F32, "ExternalInput").ap()
    hout = P.dram("hout", [128, 8, D], F32, "ExternalOutput").ap()

    hT_bf = P.sbuf("hT_bf", [128, 16, 1024], BF16)
    acc = [P.sbuf("acc%d" % n, [128, D], F32) for n in range(8)]
    st = [P.sbuf("st%d" % i, [128, D], F32) for i in range(2)]
    rw_sb = P.sbuf("rw_sb", [128, 16, 16], F32)
    rb_sb = P.sbuf("rb_sb", [128, 16], F32)
    id_sb = P.sbuf("id_sb", [128, 128], F32)
    g_sb = P.sbuf("g_sb", [128, D], F32)
    b_sb = P.sbuf("b_sb", [128, D], F32)
    P.dma("pool", rw_sb[:], rw, writes=[rw_sb])
    P.dma("pool", rb_sb[:], rb, writes=[rb_sb])
    P.dma("pool", id_sb[:], ident, writes=[id_sb])
    P.dma("pool", g_sb[:], lng, writes=[g_sb])
    P.dma("pool", b_sb[:], lnb, writes=[b_sb])
    ps = [P.psum("ps%d" % i, [128, 512], F32) for i in range(8)]
    for c in range(16):
        s = st[c % 2]
        P.dma("sp", s[:, 0:1024], hT[:, c, :], writes=[s])
        P.op("act", lambda h, s=s, c=c: h.activation(out=hT_bf[:, c, :], in_=s[:, 0:1024], func=AF.Copy), reads=[s], writes=[hT_bf])
        for half in range(2):
            P.op("pe", lambda h, s=s, c=c, half=half: h.matmul(ps[half][0:16, :], lhsT=rw_sb[:, c, :],
                                                          rhs=s[:, half * 512:(half + 1) * 512],
                                                          start=(c == 0), stop=(c == 15)),
                 reads=[s, rw_sb], writes=[ps[half]])
    lgT = P.sbuf("lgT", [16, 1024], F32)
    for half in range(2):
        P.op("dve", lambda h, half=half: h.tensor_copy(out=lgT[:, half * 512:(half + 1) * 512], in_=ps[half][0:16, :]),
             reads=[ps[half]], writes=[lgT])
    lg = P.sbuf("lg", [128, 8, 16], F32)
    for n in range(8):
        P.op("pe", lambda h, n=n: h.transpose(out=ps[2][:, n * 16:(n + 1) * 16], in_=lgT[:, n * 128:(n + 1) * 128],
                                              identity=id_sb[0:16, 0:16]), reads=[lgT, id_sb], writes=[ps[2]])
    P.op("dve", lambda h: h.tensor_copy(out=lg[:].rearrange("p n e -> p (n e)"), in_=ps[2][:, 0:128]), reads=[ps[2]], writes=[lg])
    sc = P.sbuf("sc", [128, 8, 16], F32)
    bi = P.sbuf("bi", [128, 8, 16], F32)
    t1 = P.sbuf("t1", [128, 8, 16], F32)
    m1 = P.sbuf("m1", [128, 32], F32)
    m2 = P.sbuf("m2", [128, 32], F32)
    gs = P.sbuf("gs", [128, 32], F32)
    gm = P.sbuf("gm", [128, 8], F32)
    ing = P.sbuf("ing", [128, 32], F32)
    gates = P.sbuf("gates", [128, 8, 16], F32)
    den = P.sbuf("den", [128, 8], F32)
    P.op("act", lambda h: h.activation(out=sc[:], in_=lg[:], func=AF.Sigmoid), reads=[lg], writes=[sc])
    for n in range(8):
        P.op("dve", lambda h, n=n: h.tensor_tensor(out=bi[:, n, :], in0=sc[:, n, :], in1=rb_sb[:], op=ALU.add),
             reads=[sc, rb_sb], writes=[bi])
    big = bi[:].rearrange("p n (g k) -> p (n g) k", k=4)
    t1g = t1[:].rearrange("p n (g k) -> p (n g) k", k=4)
    P.op("dve", lambda h: h.tensor_reduce(out=m1[:], in_=big, axis=AX.X, op=ALU.max), reads=[bi], writes=[m1])
    for k in range(4):
        P.op("dve", lambda h, k=k: h.tensor_tensor(out=t1g[:, :, k], in0=big[:, :, k], in1=m1[:], op=ALU.is_equal),
             reads=[bi, m1], writes=[t1])
    P.op("dve", lambda h: h.scalar_tensor_tensor(out=t1[:], in0=t1[:], scalar=-1e30, in1=bi[:], op0=ALU.mult, op1=ALU.add),
         reads=[t1, bi], writes=[t1])
    P.op("dve", lambda h: h.tensor_reduce(out=m2[:], in_=t1g, axis=AX.X, op=ALU.max), reads=[t1], writes=[m2])
    P.op("dve", lambda h: h.tensor_tensor(out=gs[:], in0=m1[:], in1=m2[:], op=ALU.add), reads=[m1, m2], writes=[gs])
    P.op("dve", lambda h: h.tensor_reduce(out=gm[:], in_=gs[:].rearrange("p (n g) -> p n g", g=4), axis=AX.X, op=ALU.max),
         reads=[gs], writes=[gm])
    for g in range(4):
        P.op("dve", lambda h, g=g: h.tensor_tensor(out=ing[:].rearrange("p (n g) -> p n g", g=4)[:, :, g],
                                                   in0=gs[:].rearrange("p (n g) -> p n g", g=4)[:, :, g], in1=gm[:],
                                                   op=ALU.is_equal), reads=[gs, gm], writes=[ing])
    gtg = gates[:].rearrange("p n (g k) -> p (n g) k", k=4)
    scg = sc[:].rearrange("p n (g k) -> p (n g) k", k=4)
    for k in range(4):
        P.op("dve", lambda h, k=k: h.tensor_tensor(out=t1g[:, :, k], in0=big[:, :, k], in1=m2[:], op=ALU.is_ge),
             reads=[bi, m2], writes=[t1])
        P.op("dve", lambda h, k=k: h.tensor_tensor(out=t1g[:, :, k], in0=t1g[:, :, k], in1=ing[:], op=ALU.mult),
             reads=[t1, ing], writes=[t1])
    P.op("dve", lambda h: h.tensor_tensor(out=gates[:], in0=t1[:], in1=sc[:], op=ALU.mult), reads=[t1, sc], writes=[gates])
    P.op("dve", lambda h: h.tensor_reduce(out=den[:], in_=gates[:], axis=AX.X, op=ALU.add), reads=[gates], writes=[den])
    P.op("dve", lambda h: h.reciprocal(out=den[:], in_=den[:]), reads=[den], writes=[den])
    for n in range(8):
        P.op("dve", lambda h, n=n: h.tensor_scalar(out=gates[:, n, :], in0=gates[:, n, :], scalar1=den[:, n:n + 1], scalar2=None,
                                                   op0=ALU.mult), reads=[gates, den], writes=[gates])
    for n in range(8):
        s = st[n % 2]
        P.dma("sp", s[:], htok[:, n, :], writes=[s])
        P.op("act", lambda h, s=s, n=n: h.activation(out=acc[n][:], in_=s[:], func=AF.Copy, scale=ALPHA), reads=[s], writes=[acc[n]])
    wg_bf = [P.sbuf("wg_bf%d" % i, [128, 16, 256], BF16) for i in range(2)]
    wu_bf = [P.sbuf("wu_bf%d" % i, [128, 16, 256], BF16) for i in range(2)]
    wd_bf = [P.sbuf("wd_bf%d" % i, [128, 2, D], BF16) for i in range(2)]
    hid = [P.sbuf("hid%d" % i, [128, 2, 1024], BF16) for i in range(2)]
    sg = [P.sbuf("sg%d" % i, [128, 512], F32) for i in range(2)]
    kk = 0
    dq = 0
    for u in range(32):
        e = u // 2; hf = u % 2
        b = u % 2
        wgv = wg[e].rearrange("(c p) f -> p c f", p=128)
        wuv = wu[e].rearrange("(c p) f -> p c f", p=128)
        wdv = wd[e].rearrange("(c p) f -> p c f", p=128)
        for q in range(2):
            for (src, dst) in ((wgv, wg_bf[b]), (wuv, wu_bf[b])):
                s = st[kk % 2]; kk += 1
                eng = "sp" if dq % 2 == 0 else "pool"; dq += 1
                P.dma(eng, s[:].rearrange("p (c f) -> p c f", f=256), src[:, q * 8:(q + 1) * 8, hf * 256:(hf + 1) * 256], writes=[s])
                if kk % 2 == 0:
                    P.op("act", lambda h, s=s, dst=dst, q=q: h.activation(out=dst[:, q * 8:(q + 1) * 8, :].rearrange("p c f -> p (c f)"),
                                                                      in_=s[:], func=AF.Copy), reads=[s], writes=[dst])
                else:
                    P.op("pool", lambda h, s=s, dst=dst, q=q: h.tensor_copy(out=dst[:, q * 8:(q + 1) * 8, :].rearrange("p c f -> p (c f)"),
                                                                       in_=s[:]), reads=[s], writes=[dst])
        for q in range(2):
            s = st[kk % 2]; kk += 1
            eng = "sp" if dq % 2 == 0 else "pool"; dq += 1
            P.dma(eng, s[:], wdv[:, hf * 2 + q, :], writes=[s])
            P.op("pool", lambda h, s=s, q=q, b=b: h.tensor_copy(out=wd_bf[b][:, q, :], in_=s[:]), reads=[s], writes=[wd_bf[b]])
        hb = hid[b]
        for fb in range(2):
            for th in range(2):
                pg = ps[(fb * 2 + th) % 2]
                pu = ps[2 + (fb * 2 + th) % 2]
                for c in range(16):
                    P.op("pe", lambda h, pg=pg, c=c, fb=fb, th=th, b=b: h.matmul(pg[:], lhsT=wg_bf[b][:, c, fb * 128:(fb + 1) * 128],
                                                                         rhs=hT_bf[:, c, th * 512:(th + 1) * 512],
                                                                         start=(c == 0), stop=(c == 15)),
                         reads=[wg_bf[b], hT_bf], writes=[pg])
                for c in range(16):
                    P.op("pe", lambda h, pu=pu, c=c, fb=fb, th=th, b=b: h.matmul(pu[:], lhsT=wu_bf[b][:, c, fb * 128:(fb + 1) * 128],
                                                                         rhs=hT_bf[:, c, th * 512:(th + 1) * 512],
                                                                         start=(c == 0), stop=(c == 15)),
                         reads=[wu_bf[b], hT_bf], writes=[pu])
                sgb = sg[(fb * 2 + th) % 2]
                P.op("act", lambda h, pg=pg, sgb=sgb: h.activation(out=sgb[:], in_=pg[:], func=AF.Silu), reads=[pg], writes=[sgb])
                P.op("dve", lambda h, pu=pu, sgb=sgb, hb=hb, fb=fb, th=th: h.tensor_tensor(
                    out=hb[:, fb, th * 512:(th + 1) * 512], in0=sgb[:], in1=pu[:], op=ALU.mult), reads=[sgb, pu], writes=[hb])
        for n in range(8):
            for db in range(4):
                pd = ps[4 + (n * 4 + db) % 4]
                for fc in range(2):
                    P.op("pe", lambda h, pd=pd, fc=fc, n=n, db=db, hb=hb, b=b: h.matmul(pd[:], lhsT=hb[:, fc, n * 128:(n + 1) * 128],
                                                                                rhs=wd_bf[b][:, fc, db * 512:(db + 1) * 512],
                                                                                start=(fc == 0), stop=(fc == 1)),
                         reads=[hb, wd_bf[b]], writes=[pd])
                P.op("dve", lambda h, pd=pd, n=n, db=db, e=e: h.scalar_tensor_tensor(
                    out=acc[n][:, db * 512:(db + 1) * 512], in0=pd[:], scalar=gates[:, n, e:e + 1],
                    in1=acc[n][:, db * 512:(db + 1) * 512], op0=ALU.mult, op1=ALU.add), reads=[pd, gates, acc[n]], writes=[acc[n]])
    scr = P.sbuf("scr", [128, D], F32)
    stats = P.sbuf("stats", [128, 24], F32)
    mv = P.sbuf("mv", [128, 2], F32)
    rstd = P.sbuf("rstd", [128, 1], F32)
    for n in range(8):
        ob = st[n % 2]
        ln_tile(P, acc[n][:], acc[n], scr, g_sb, b_sb, ob, "k2", stats, mv, rstd)
        P.dma("sp", hout[:, n, :], ob[:], reads=[ob], is_output=True)
    return P.finish()


_CACHE = {}


def _get(name, fn, *a):
    key = (name,) + a
    if key not in _CACHE:
        _CACHE[key] = fn(*a)
    return _CACHE[key]


def tokmajor(h):
    return [np.ascontiguousarray(h[c * 1024:(c + 1) * 1024].reshape(8, 128, -1).transpose(1, 0, 2)) for c in range(8)]


def untok(res, key):
    return np.concatenate([r[key].transpose(1, 0, 2).reshape(1024, -1) for r in res], axis=0)


def featmajor(o):
    E = o.shape[1]
    return [np.ascontiguousarray(o[c * 1024:(c + 1) * 1024].T.reshape(E // 128, 128, 1024).transpose(1, 0, 2)) for c in range(8)]


def bc(v):
    return np.ascontiguousarray(np.broadcast_to(np.asarray(v, np.float32)[None, :], (128, v.shape[-1])))


def run_k1(o, xres, w_out, g, b):
    E = o.shape[1]
    nc = _get("k1", build_k1, E // 128)
    oT = featmajor(o); xr = tokmajor(xres)
    wl = np.ascontiguousarray(w_out.reshape(E // 128, 128, D).transpose(1, 0, 2))
    maps = [{"oT": oT[c], "xres": xr[c], "w": wl, "lng": bc(g), "lnb": bc(b)} for c in range(8)]
    res = run_bass_kernel_spmd(nc, maps, core_ids=list(range(8)))
    return untok(res.results, "hout")


def run_k2(h, rw, rb, wg, wu, wd, g, b):
    nc = _get("k2", build_k2)
    hT = featmajor(h); ht = tokmajor(h)
    rwl = np.ascontiguousarray(rw.reshape(16, 128, 16).transpose(1, 0, 2))
    ident = np.eye(128, dtype=np.float32)
    maps = [{"hT": hT[c], "htok": ht[c], "rw": rwl, "rb": bc(rb), "wg": wg, "wu": wu, "wd": wd, "lng": bc(g), "lnb": bc(b),
             "ident": ident} for c in range(8)]
    res = run_bass_kernel_spmd(nc, maps, core_ids=list(range(8)))
    return untok(res.results, "hout")


NEG = -1e30
SCALE = 128 ** -0.5
DIL = ((128, 1), (512, 4), (2048, 16))


def build_a1():
    P = Prog()
    S = 2048
    hT = P.dram("hT", [128, 16, S], F32, "ExternalInput").ap()
    wqkv = P.dram("wqkv", [8, 128, 16, 640], F32, "ExternalInput").ap()
    ctab = P.dram("ctab", [128, S], F32, "ExternalInput").ap()
    stab = P.dram("stab", [128, S], F32, "ExternalInput").ap()
    masks = P.dram("masks", [128, 512], F32, "ExternalInput").ap()
    identb = P.dram("identb", [128, 128], F32, "ExternalInput").ap()
    oout = P.dram("oout", [128, 16, 4, 128], F32, "ExternalOutput").ap()
    scr_t = P.dram("scr", [2, 3, S, 130], F32, "Internal")
    scr = scr_t.ap()
    scrb = P.track(scr_t, "scr")

    hT_bf = P.sbuf("hT_bf", [128, 16, S], BF16)
    st = [P.sbuf("st%d" % i, [128, 4 * 640], F32) for i in range(2)]
    CT = P.sbuf("CT", [128, S], F32)
    ST = P.sbuf("ST", [128, S], F32)
    mk = P.sbuf("mk", [128, 512], F32)
    idf = P.sbuf("idf", [128, 128], F32)
    idb = P.sbuf("idb", [128, 128], BF16)
    P.dma("pool", CT[:], ctab, writes=[CT])
    P.dma("pool", ST[:], stab, writes=[ST])
    P.dma("pool", mk[:], masks, writes=[mk])
    P.dma("pool", idf[:], identb, writes=[idf])
    P.op("dve", lambda h: h.tensor_copy(out=idb[:], in_=idf[:]), reads=[idf], writes=[idb])
    for c in range(16):
        s = st[c % 2]
        P.dma("sp", s[:, 0:S], hT[:, c, :], writes=[s])
        if c % 2 == 0:
            P.op("act", lambda h, s=s, c=c: h.activation(out=hT_bf[:, c, :], in_=s[:, 0:S], func=AF.Copy), reads=[s], writes=[hT_bf])
        else:
            P.op("dve", lambda h, s=s, c=c: h.tensor_copy(out=hT_bf[:, c, :], in_=s[:, 0:S]), reads=[s], writes=[hT_bf])
    ps = [P.psum("ps%d" % i, [128, 512], F32) for i in range(4)]
    pst = [P.psum("pst%d" % i, [128, 1024], BF16) for i in range(2)]
    pso = [P.psum("pso%d" % i, [128, 512], F32) for i in range(2)]
    w_bf = P.sbuf("w_bf", [128, 16, 640], BF16)
    qf = P.sbuf("qf", [128, S], F32)
    t1 = P.sbuf("t1", [128, S], F32)
    t2 = P.sbuf("t2", [128, S], F32)
    q_bf = P.sbuf("q_bf", [128, S], BF16)
    k_bf = P.sbuf("k_bf", [128, S], BF16)
    v_bf = P.sbuf("v_bf", [128, 16, 128], BF16)
    sm = [P.sbuf("sm%d" % i, [128, S], F32) for i in range(2)]
    pp = [P.sbuf("pp%d" % i, [128, S], BF16) for i in range(2)]
    pT = [P.sbuf("pT%d" % i, [128, 16, 128], BF16) for i in range(2)]
    mx = [P.sbuf("mx%d" % i, [128, 1], F32) for i in range(2)]
    nmx = [P.sbuf("nmx%d" % i, [128, 1], F32) for i in range(2)]
    pk = [P.sbuf("pk%d" % i, [128, 130], F32) for i in range(2)]
    km = P.sbuf("km", [128, 8], F32)
    gt = P.sbuf("gt", [128, 16, 8], F32)
    g8 = P.sbuf("g8", [128, 8], F32)
    top8 = P.sbuf("top8", [128, 8], F32)
    bias = P.sbuf("bias", [128, 8], F32)
    rden = P.sbuf("rden", [128, 1], F32)
    ot = [P.sbuf("ot%d" % i, [128, 128], F32) for i in range(2)]
    ucount = [0]

    def project(hs, d):
        for q in range(4):
            s = st[q % 2]
            P.dma("sp" if q % 2 == 0 else "pool", s[:].rearrange("p (c f) -> p c f", f=640), wqkv[hs, :, q * 4:(q + 1) * 4, :], writes=[s])
            if q % 2 == 0:
                P.op("act", lambda h, s=s, q=q: h.activation(out=w_bf[:, q * 4:(q + 1) * 4, :].rearrange("p c f -> p (c f)"), in_=s[:], func=AF.Copy),
                     reads=[s], writes=[w_bf])
            else:
                P.op("dve", lambda h, s=s, q=q: h.tensor_copy(out=w_bf[:, q * 4:(q + 1) * 4, :].rearrange("p c f -> p (c f)"), in_=s[:]),
                     reads=[s], writes=[w_bf])
        for which in range(2):
            for tb in range(4):
                pa = ps[(tb * 2) % 4]; pb = ps[(tb * 2 + 1) % 4]
                for (pz, col) in ((pa, which * 256), (pb, which * 256 + 128)):
                    for c in range(16):
                        P.op("pe", lambda h, pz=pz, c=c, col=col, tb=tb: h.matmul(pz[:], lhsT=w_bf[:, c, col:col + 128],
                                                                            rhs=hT_bf[:, c, tb * 512:(tb + 1) * 512],
                                                                            start=(c == 0), stop=(c == 15)),
                             reads=[w_bf, hT_bf], writes=[pz])
                sl = slice(tb * 512, (tb + 1) * 512)
                P.op("dve", lambda h, pa=pa, sl=sl: h.tensor_tensor(out=t1[:, sl], in0=pa[:], in1=CT[:, sl], op=ALU.mult),
                     reads=[pa, CT], writes=[t1])
                P.op("dve", lambda h, pb=pb, sl=sl: h.tensor_tensor(out=t2[:, sl], in0=pb[:], in1=ST[:, sl], op=ALU.mult),
                     reads=[pb, ST], writes=[t2])
                if which == 0:
                    P.op("pool", lambda h, sl=sl: h.tensor_tensor(out=qf[:, sl], in0=t1[:, sl], in1=t2[:, sl], op=ALU.add),
                         reads=[t1, t2], writes=[qf])
                    P.op("act", lambda h, sl=sl: h.activation(out=q_bf[:, sl], in_=qf[:, sl], func=AF.Copy), reads=[qf], writes=[q_bf])
                else:
                    P.op("pool", lambda h, sl=sl: h.tensor_tensor(out=t1[:, sl], in0=t1[:, sl], in1=t2[:, sl], op=ALU.add),
                         reads=[t1, t2], writes=[t1])
                    P.op("act", lambda h, sl=sl: h.activation(out=k_bf[:, sl], in_=t1[:, sl], func=AF.Copy), reads=[t1], writes=[k_bf])
        nb = S // (d * 128)
        for r in range(d):
            for i in range(nb):
                u = r * nb + i
                pz = ps[u % 4]
                t0 = r + d * 128 * i
                for c in range(16):
                    P.op("pe", lambda h, pz=pz, c=c, t0=t0, d=d: h.matmul(pz[:, 0:128], lhsT=hT_bf[:, c, t0:t0 + 127 * d + 1:d],
                                                                     rhs=w_bf[:, c, 512:640], start=(c == 0), stop=(c == 15)),
                         reads=[hT_bf, w_bf], writes=[pz])
                P.op("act" if u % 2 == 0 else "dve",
                     (lambda h, pz=pz, u=u: h.activation(out=v_bf[:, u, :], in_=pz[:, 0:128], func=AF.Copy)) if u % 2 == 0 else
                     (lambda h, pz=pz, u=u: h.tensor_copy(out=v_bf[:, u, :], in_=pz[:, 0:128])),
                     reads=[pz], writes=[v_bf])

    def softmax_pv(smb, ppb, pTb, nk, vtiles, mxb, nmxb, den_ap, den_buf):
        k = ucount[0]; ucount[0] += 1
        W = nk * 128
        P.op("dve", lambda h: h.tensor_reduce(out=mxb[:], in_=smb[:, 0:W], axis=AX.X, op=ALU.max), reads=[smb], writes=[mxb])
        P.op("dve", lambda h: h.tensor_scalar(out=nmxb[:], in0=mxb[:], scalar1=-1.0, scalar2=None, op0=ALU.mult), reads=[mxb], writes=[nmxb])
        P.op("act", lambda h: h.activation(out=ppb[:, 0:W], in_=smb[:, 0:W], func=AF.Exp, bias=nmxb[:, 0:1], scale=1.0,
                                           accum_out=den_ap), reads=[smb, nmxb], writes=[ppb, den_buf])
        po = pso[k % 2]
        for g0 in range(0, nk, 8):
            g1 = min(nk, g0 + 8)
            ptb = pst[(g0 // 8) % 2]
            for j in range(g0, g1):
                P.op("pe", lambda h, j=j, g0=g0, ptb=ptb: h.transpose(out=ptb[:, (j - g0) * 128:(j - g0 + 1) * 128],
                                                                  in_=ppb[:, j * 128:(j + 1) * 128], identity=idb[:]),
                     reads=[ppb, idb], writes=[ptb])
            P.op("dve" if (g0 // 8) % 2 == 0 else "pool" if False else "dve",
                 lambda h, g0=g0, g1=g1, ptb=ptb: h.tensor_copy(out=pTb[:, g0:g1, :].rearrange("p a b -> p (a b)"),
                                                             in_=ptb[:, 0:(g1 - g0) * 128]), reads=[ptb], writes=[pTb])
        for j in range(nk):
            P.op("pe", lambda h, j=j, po=po: h.matmul(po[:, 0:128], lhsT=pTb[:, j, :], rhs=v_bf[:, vtiles[j], :],
                                                      start=(j == 0), stop=(j == nk - 1)), reads=[pTb, v_bf], writes=[po])
        return po

    for hgl in range(2):
        for g, (win, d) in enumerate(DIL):
            hs = g * 2 + hgl
            project(hs, d)
            nb = S // (d * 128)
            for r in range(d):
                for i in range(nb):
                    k = ucount[0]
                    smb = sm[k % 2]; ppb = pp[k % 2]; pTb = pT[k % 2]; pkb = pk[k % 2]
                    t0 = r + d * 128 * i
                    pz = ps[k % 4]
                    if i == 0:
                        nk = 1
                        P.op("pe", lambda h, pz=pz, t0=t0, d=d: h.matmul(pz[:, 0:128], lhsT=q_bf[:, t0:t0 + 127 * d + 1:d],
                                                                    rhs=k_bf[:, t0:t0 + 127 * d + 1:d], start=True, stop=True),
                             reads=[q_bf, k_bf], writes=[pz])
                        P.op("dve", lambda h, pz=pz, smb=smb: h.scalar_tensor_tensor(out=smb[:, 0:128], in0=pz[:, 0:128], scalar=SCALE,
                                                                                 in1=mk[:, 128:256], op0=ALU.mult, op1=ALU.add),
                             reads=[pz, mk], writes=[smb])
                        vt = [r * nb + i]
                    else:
                        nk = 2
                        k0 = t0 - d * 128
                        P.op("pe", lambda h, pz=pz, t0=t0, k0=k0, d=d: h.matmul(pz[:, 0:256], lhsT=q_bf[:, t0:t0 + 127 * d + 1:d],
                                                                          rhs=k_bf[:, k0:k0 + 255 * d + 1:d], start=True, stop=True),
                             reads=[q_bf, k_bf], writes=[pz])
                        P.op("dve", lambda h, pz=pz, smb=smb: h.scalar_tensor_tensor(out=smb[:, 0:256], in0=pz[:, 0:256], scalar=SCALE,
                                                                                 in1=mk[:, 0:256], op0=ALU.mult, op1=ALU.add),
                             reads=[pz, mk], writes=[smb])
                        vt = [r * nb + i - 1, r * nb + i]
                    po = softmax_pv(smb, ppb, pTb, nk, vt, mx[k % 2], nmx[k % 2], pkb[:, 129:130], pkb)
                    P.op("act", lambda h, po=po, pkb=pkb: h.activation(out=pkb[:, 0:128], in_=po[:, 0:128], func=AF.Copy),
                         reads=[po], writes=[pkb])
                    P.op("dve", lambda h, pkb=pkb, k=k: h.tensor_copy(out=pkb[:, 128:129], in_=mx[k % 2][:]), reads=[mx[k % 2]], writes=[pkb])
                    P.dma("sp", scr[hgl, g, t0:t0 + 127 * d + 1:d, :], pkb[:], reads=[pkb], writes=[scrb])
    mg = [P.sbuf("mg%d" % i, [128, 3, 130], F32) for i in range(2)]
    M = P.sbuf("M", [128, 1], F32)
    e3 = P.sbuf("e3", [128, 3], F32)
    z3 = P.sbuf("z3", [128, 3], F32)
    Z = P.sbuf("Z", [128, 1], F32)
    for hgl in range(2):
        for n in range(16):
            k = hgl * 16 + n
            m = mg[k % 2]; o = ot[k % 2]
            P.dma("pool", m[:], scr[hgl, :, n * 128:(n + 1) * 128, :].rearrange("g t f -> t g f"), reads=[scrb], writes=[m])
            P.op("dve", lambda h, m=m: h.tensor_reduce(out=M[:], in_=m[:, :, 128], axis=AX.X, op=ALU.max), reads=[m], writes=[M])
            P.op("dve", lambda h: h.tensor_scalar(out=M[:], in0=M[:], scalar1=-1.0, scalar2=None, op0=ALU.mult), reads=[M], writes=[M])
            P.op("act", lambda h, m=m: h.activation(out=e3[:], in_=m[:, :, 128], func=AF.Exp, bias=M[:, 0:1], scale=1.0),
                 reads=[m, M], writes=[e3])
            P.op("dve", lambda h, m=m: h.tensor_tensor(out=z3[:], in0=e3[:], in1=m[:, :, 129], op=ALU.mult), reads=[e3, m], writes=[z3])
            P.op("dve", lambda h: h.tensor_reduce(out=Z[:], in_=z3[:], axis=AX.X, op=ALU.add), reads=[z3], writes=[Z])
            P.op("dve", lambda h: h.reciprocal(out=Z[:], in_=Z[:]), reads=[Z], writes=[Z])
            P.op("dve", lambda h: h.tensor_scalar(out=e3[:], in0=e3[:], scalar1=Z[:, 0:1], scalar2=None, op0=ALU.mult), reads=[e3, Z], writes=[e3])
            P.op("dve", lambda h, m=m, o=o: h.tensor_scalar(out=o[:], in0=m[:, 0, 0:128], scalar1=e3[:, 0:1], scalar2=None, op0=ALU.mult),
                 reads=[m, e3], writes=[o])
            for g in (1, 2):
                P.op("dve", lambda h, m=m, o=o, g=g: h.scalar_tensor_tensor(out=o[:], in0=m[:, g, 0:128], scalar=e3[:, g:g + 1], in1=o[:],
                                                                         op0=ALU.mult, op1=ALU.add), reads=[m, e3, o], writes=[o])
            P.dma("sp", oout[:, n, hgl, :], o[:], reads=[o], is_output=True)
    for hl in range(2):
        hs = 6 + hl
        project(hs, 1)
        P.op("dve", lambda h: h.tensor_reduce(out=km[:], in_=t1[:].rearrange("p (b t) -> p b t", t=256), axis=AX.X, op=ALU.add),
             reads=[t1], writes=[km])
        P.op("dve", lambda h: h.tensor_scalar(out=km[:], in0=km[:], scalar1=1.0 / 256, scalar2=None, op0=ALU.mult), reads=[km], writes=[km])
        for n in range(16):
            P.op("pe", lambda h, n=n: h.matmul(ps[0][:, n * 8:(n + 1) * 8], lhsT=qf[:, n * 128:(n + 1) * 128], rhs=km[:],
                                               start=True, stop=True), reads=[qf, km], writes=[ps[0]])
        P.op("dve", lambda h: h.tensor_copy(out=gt[:].rearrange("p n e -> p (n e)"), in_=ps[0][:, 0:128]), reads=[ps[0]], writes=[gt])
        for n in range(16):
            qb = n // 2
            k = ucount[0]
            smb = sm[k % 2]; ppb = pp[k % 2]; pTb = pT[k % 2]
            if qb >= 4:
                P.op("dve", lambda h: h.memset(g8[:], NEG), writes=[g8])
                P.op("dve", lambda h, n=n, qb=qb: h.tensor_copy(out=g8[:, 0:qb], in_=gt[:, n, 0:qb]), reads=[gt], writes=[g8])
                P.op("dve", lambda h: h.max(out=top8[:], in_=g8[:]), reads=[g8], writes=[top8])
                P.op("dve", lambda h: h.tensor_scalar(out=bias[:], in0=g8[:], scalar1=top8[:, 2:3], scalar2=None, op0=ALU.is_ge),
                     reads=[g8, top8], writes=[bias])
                P.op("dve", lambda h: h.tensor_scalar(out=bias[:], in0=bias[:], scalar1=-1.0, scalar2=1e30, op0=ALU.add, op1=ALU.mult),
                     reads=[bias], writes=[bias])
            else:
                P.op("dve", lambda h: h.memset(bias[:], 0.0), writes=[bias])
            for j in range(qb):
                pz = ps[j % 4]
                P.op("pe", lambda h, pz=pz, n=n, j=j: h.matmul(pz[:, 0:256], lhsT=q_bf[:, n * 128:(n + 1) * 128],
                                                            rhs=k_bf[:, j * 256:(j + 1) * 256], start=True, stop=True),
                     reads=[q_bf, k_bf], writes=[pz])
                P.op("dve", lambda h, pz=pz, j=j, smb=smb: h.tensor_scalar(out=smb[:, j * 256:(j + 1) * 256], in0=pz[:, 0:256], scalar1=SCALE,
                                                                       scalar2=bias[:, j:j + 1], op0=ALU.mult, op1=ALU.add),
                     reads=[pz, bias], writes=[smb])
            pz = ps[qb % 4]
            own = 128 * (1 + n % 2)
            P.op("pe", lambda h, pz=pz, n=n, qb=qb, own=own: h.matmul(pz[:, 0:own], lhsT=q_bf[:, n * 128:(n + 1) * 128],
                                                                   rhs=k_bf[:, qb * 256:qb * 256 + own], start=True, stop=True),
                 reads=[q_bf, k_bf], writes=[pz])
            mo = (128, 256) if n % 2 == 0 else (256, 512)
            P.op("dve", lambda h, pz=pz, qb=qb, own=own, mo=mo, smb=smb: h.scalar_tensor_tensor(
                out=smb[:, qb * 256:qb * 256 + own], in0=pz[:, 0:own], scalar=SCALE, in1=mk[:, mo[0]:mo[1]], op0=ALU.mult, op1=ALU.add),
                 reads=[pz, mk], writes=[smb])
            nk = 2 * qb + 1 + n % 2
            po = softmax_pv(smb, ppb, pTb, nk, list(range(nk)), mx[k % 2], nmx[k % 2], rden[:, 0:1], rden)
            o = ot[k % 2]
            P.op("dve", lambda h: h.reciprocal(out=rden[:], in_=rden[:]), reads=[rden], writes=[rden])
            P.op("dve", lambda h, po=po, o=o: h.tensor_scalar(out=o[:], in0=po[:, 0:128], scalar1=rden[:, 0:1], scalar2=None, op0=ALU.mult),
                 reads=[po, rden], writes=[o])
            P.dma("sp", oout[:, n, 2 + hl, :], o[:], reads=[o], is_output=True)
    return P.finish()


def a1_consts():
    S = 2048
    inv = 10000.0 ** (-np.arange(0, 128, 2, dtype=np.float32) / 128)
    ang = np.arange(S, dtype=np.float32)[:, None] * inv[None, :]
    cos = np.cos(ang).T.astype(np.float32); sin = np.sin(ang).T.astype(np.float32)
    ctab = np.concatenate([cos, cos], 0); stab = np.concatenate([-sin, sin], 0)
    q = np.arange(128)[:, None]; c = np.arange(128)[None, :]
    prev = np.where(c >= q, 0.0, NEG); own = np.where(c <= q, 0.0, NEG)
    masks = np.concatenate([prev, own, np.zeros((128, 128)), own], 1).astype(np.float32)
    return np.ascontiguousarray(ctab), np.ascontiguousarray(stab), masks


def run_a1(h, w_in):
    nc = _get("a1", build_a1)
    ctab, stab, masks = a1_consts()
    ident = np.eye(128, dtype=np.float32)
    sw = np.concatenate([np.arange(64, 128), np.arange(64)])
    maps = []
    for core in range(8):
        b, half = core // 2, core % 2
        hT = np.ascontiguousarray(h[b].T.reshape(16, 128, 2048).transpose(1, 0, 2))
        sets = []
        for hgl in range(2):
            for g in range(3):
                hd = g * 4 + half * 2 + hgl
                sets.append((hd * 128, 1536 + hd * 128, 3072 + hd * 128))
        order = [sets[hgl * 3 + g] for g in range(3) for hgl in range(2)]
        for hl in range(2):
            hd = half * 2 + hl
            order.append((4608 + hd * 128, 5120 + hd * 128, 5632 + hd * 128))
        wl = np.empty((8, 128, 16, 640), np.float32)
        for hs, (qo, ko, vo) in enumerate(order):
            wq = w_in[:, qo:qo + 128]; wk = w_in[:, ko:ko + 128]; wv = w_in[:, vo:vo + 128]
            cat = np.concatenate([wq, wq[:, sw], wk, wk[:, sw], wv], 1)
            wl[hs] = cat.reshape(16, 128, 640).transpose(1, 0, 2)
        maps.append({"hT": hT, "wqkv": wl, "ctab": ctab, "stab": stab, "masks": masks, "identb": ident})
    res = run_bass_kernel_spmd(nc, maps, core_ids=list(range(8)))
    o = np.empty((4, 2048, 8, 128), np.float32)
    for core in range(8):
        b, half = core // 2, core % 2
        r = res.results[core]["oout"]
        r = r.transpose(1, 0, 2, 3).reshape(2048, 4, 128)
        o[b, :, half * 2:half * 2 + 2] = r[:, 0:2]
        o[b, :, 4 + half * 2:4 + half * 2 + 2] = r[:, 2:4]
    return o.reshape(4, 2048, 1024)


class View(Buf):
    def __init__(self, t, lo, hi, name):
        Buf.__init__(self, t, name)
        self.lo = lo
        self.hi = hi

    def __getitem__(self, idx):
        return self.t[:, self.lo:self.hi][idx]


class BankView(View):
    def __init__(self, parent, lo, hi, name):
        self.__dict__["parent"] = parent
        View.__init__(self, parent.t, lo, hi, name)
        self.excl = True
        parent.excl = True

    w = property(lambda self: self.parent.w, lambda self, v: setattr(self.parent, "w", v))
    r = property(lambda self: self.parent.r, lambda self, v: setattr(self.parent, "r", v))


RMS_EPS = 1e-6


class _Stop(Exception):
    pass


def build_a0(stage="full"):
    P = Prog()
    try:
        _build_a0(P, stage)
    except _Stop:
        pass
    return P.finish()


def _build_a0(P, stage):
    S = 2048
    hT = P.dram("hT", [128, 16, S], F32, "ExternalInput").ap()
    wg_fm = P.dram("wg_fm", [4, 128, 16, 512], F32, "ExternalInput").ap()
    wh_fm = P.dram("wh_fm", [4, 128, 16, 512], F32, "ExternalInput").ap()
    wtm = P.dram("wtm", [3, 128, 16, 512], F32, "ExternalInput").ap()
    wgate = P.dram("wgate", [128, 16, 8], F32, "ExternalInput").ap()
    cw = P.dram("cw", [128, 4, 3, 4], F32, "ExternalInput").ap()
    hp = P.dram("hp", [128, 8], F32, "ExternalInput").ap()
    norms = P.dram("norms", [128, 256], F32, "ExternalInput").ap()
    lbl = P.dram("lbl", [128, 4, 3], F32, "ExternalInput").ap()
    cm = P.dram("cm", [128, 5, 128], F32, "ExternalInput").ap()
    rmask = P.dram("rmask", [128, S], F32, "ExternalInput").ap()
    oout = P.dram("oout", [128, 16, 8, 128], F32, "ExternalOutput").ap()

    HO = 8
    hT_bf = P.sbuf("hT_bf", [128, 16, S + HO], BF16)
    F1 = P.sbuf("F1", [128, S + 4], F32)
    F2 = P.sbuf("F2", [128, S + 4], F32)
    F3 = P.sbuf("F3", [128, S + 4], F32)
    cms = P.sbuf("cms", [128, 5, 128], F32)
    idf = View(cms.t, 0, 128, "idf"); onesf = View(cms.t, 128, 256, "onesf")
    cmsf = cms.t[:].rearrange("p a b -> p (a b)")
    for v in (idf, onesf):
        v.t = cmsf
    MBs = View(cmsf, 256, 384, "MBs"); MBi = View(cmsf, 384, 512, "MBi"); Utri = View(cmsf, 512, 640, "Utri")
    consts = [idf, onesf, MBs, MBi, Utri]
    idb = P.sbuf("idb", [128, 128], BF16)
    onesb = P.sbuf("onesb", [128, 128], BF16)
    zeros = P.sbuf("zeros", [128, 128], F32)
    rm = P.sbuf("rm", [128, S], F32)
    cws = P.sbuf("cws", [128, 48], F32)
    hps = P.sbuf("hps", [128, 8], F32)
    nrm = P.sbuf("nrm", [128, 256], F32)
    lbs = P.sbuf("lbs", [128, 12], F32)
    tok = P.dma("pool", cms[:], cm, writes=[cms])
    for v in consts:
        v.w = tok
    P.dma("pool", rm[:], rmask, writes=[rm])
    P.dma("pool", cws[:], cw.rearrange("p a b c -> p (a b c)"), writes=[cws])
    P.dma("pool", hps[:], hp, writes=[hps])
    P.dma("pool", nrm[:], norms, writes=[nrm])
    P.dma("pool", lbs[:], lbl.rearrange("p a b -> p (a b)"), writes=[lbs])
    P.op("dve", lambda h: h.tensor_copy(out=idb[:], in_=idf[:]), reads=[idf], writes=[idb])
    P.op("dve", lambda h: h.tensor_copy(out=onesb[:], in_=onesf[:]), reads=[onesf], writes=[onesb])
    P.op("dve", lambda h: h.memset(zeros[:], 0.0), writes=[zeros])
    for c in range(16):
        P.op("pool", lambda h, c=c: h.memset(hT_bf[:, c, 0:HO], 0.0), writes=[hT_bf])
    for c in range(16):
        s = F1 if c % 2 == 0 else F2
        P.dma("sp", s[:, 0:S], hT[:, c, :], writes=[s])
        if c % 2 == 0:
            P.op("act", lambda h, s=s, c=c: h.activation(out=hT_bf[:, c, HO:HO + S], in_=s[:, 0:S], func=AF.Copy), reads=[s], writes=[hT_bf])
        else:
            P.op("dve", lambda h, s=s, c=c: h.tensor_copy(out=hT_bf[:, c, HO:HO + S], in_=s[:, 0:S]), reads=[s], writes=[hT_bf])
    bank = [P.psum("bk%d" % i, [128, 512], F32) for i in range(6)]
    bankb = P.psum("bkb", [128, 1024], BF16)
    bank7 = P.psum("bk7", [128, 512], F32)
    pA, pB = bank[0], bank[1]
    Q = lambda b, i, nm: BankView(b, i * 128, (i + 1) * 128, nm)
    pR = Q(bank[2], 0, "pR"); pKQ = BankView(bank[2], 128, 384, "pKQ"); pLT = Q(bank[2], 3, "pLT")
    pN = [Q(bank[3], 0, "pN0"), Q(bank7, 0, "pN1"), Q(bank[3], 1, "pN2"), Q(bank7, 1, "pN3")]
    pU = Q(bank[4], 0, "pU"); pW = Q(bank[4], 1, "pW"); p1 = Q(bank[4], 2, "p1"); p2 = Q(bank[4], 3, "p2")
    p3 = Q(bank[5], 0, "p3"); hPp = Q(bank[5], 1, "hPp"); hOp = Q(bank[5], 2, "hOp"); hSp = Q(bank[5], 3, "hSp")
    pT = [BankView(bankb, i * 128, (i + 1) * 128, "pT%d" % i) for i in range(8)]

    st = [P.sbuf("st%d" % i, [128, 2 * 512], F32) for i in range(2)]
    w_bf = P.sbuf("w_bf", [128, 16, 512], BF16)
    TM1 = P.sbuf("TM1", [128, 16, 512], BF16)
    TM2 = P.sbuf("TM2", [128, 16, 512], BF16)
    sq = [0]

    def load_w(src, ncol):
        for q in range(8):
            s = st[sq[0] % 2]; sq[0] += 1
            P.dma("sp" if q % 2 == 0 else "pool", s[:, 0:2 * ncol].rearrange("p (c f) -> p c f", f=ncol), src[:, q * 2:(q + 1) * 2, :], writes=[s])
            if q % 2 == 0:
                P.op("act", lambda h, s=s, q=q: h.activation(out=w_bf[:, q * 2:(q + 1) * 2, 0:ncol],
                                                          in_=s[:, 0:2 * ncol].rearrange("p (c f) -> p c f", f=ncol), func=AF.Copy),
                     reads=[s], writes=[w_bf])
            else:
                P.op("dve", lambda h, s=s, q=q: h.tensor_copy(out=w_bf[:, q * 2:(q + 1) * 2, 0:ncol],
                                                           in_=s[:, 0:2 * ncol].rearrange("p (c f) -> p c f", f=ncol)),
                     reads=[s], writes=[w_bf])

    def proj_fm(col, evac, sh=0):
        for tb in range(4):
            pz = pA if tb % 2 == 0 else pB
            for c in range(16):
                P.op("pe", lambda h, pz=pz, c=c, tb=tb: h.matmul(pz[:], lhsT=w_bf[:, c, col:col + 128], rhs=hT_bf[:, c, HO - sh + tb * 512:HO - sh + (tb + 1) * 512],
                                                          start=(c == 0), stop=(c == 15)), reads=[w_bf, hT_bf], writes=[pz])
            evac(pz, tb)

    def proj_tm(dst, func):
        for n in range(16):
            pz = pA if n % 2 == 0 else pB
            for c in range(16):
                P.op("pe", lambda h, pz=pz, c=c, n=n: h.matmul(pz[:], lhsT=hT_bf[:, c, HO + n * 128:HO + (n + 1) * 128], rhs=w_bf[:, c, :],
                                                        start=(c == 0), stop=(c == 15)), reads=[hT_bf, w_bf], writes=[pz])
            P.op("act", lambda h, pz=pz, n=n: h.activation(out=dst[:, n, :], in_=pz[:], func=func), reads=[pz], writes=[dst])

    wgs = P.sbuf("wgs", [128, 16, 8], F32)
    wgb = P.sbuf("wgb", [128, 16, 8], BF16)
    P.dma("pool", wgs[:], wgate, writes=[wgs])
    P.op("dve", lambda h: h.tensor_copy(out=wgb[:], in_=wgs[:]), reads=[wgs], writes=[wgb])
    for n in range(16):
        for c in range(16):
            P.op("pe", lambda h, c=c, n=n: h.matmul(bank7[:, n * 8:(n + 1) * 8], lhsT=hT_bf[:, c, HO + n * 128:HO + (n + 1) * 128], rhs=wgb[:, c, :],
                                               start=(c == 0), stop=(c == 15)), reads=[hT_bf, wgb], writes=[bank7])
    graw = P.sbuf("graw", [128, 16, 8], F32)
    beta = P.sbuf("beta", [128, 16, 4], F32)
    gg = P.sbuf("gg", [128, 16, 4], F32)
    gcum = P.sbuf("gcum", [128, 16, 4], F32)
    egc = P.sbuf("egc", [128, 16, 4], F32)
    beg = P.sbuf("beg", [128, 16, 4], F32)
    negA = P.sbuf("negA", [128, 4], F32)
    P.op("dve", lambda h: h.tensor_copy(out=graw[:].rearrange("p n e -> p (n e)"), in_=bank7[:, 0:128]), reads=[bank7], writes=[graw])
    P.op("act", lambda h: h.activation(out=beta[:], in_=graw[:, :, 0:4], func=AF.Sigmoid), reads=[graw], writes=[beta])
    P.op("act", lambda h: h.activation(out=negA[:], in_=hps[:, 0:4], func=AF.Exp), reads=[hps], writes=[negA])
    P.op("dve", lambda h: h.tensor_scalar(out=negA[:], in0=negA[:], scalar1=-1.0, scalar2=None, op0=ALU.mult), reads=[negA], writes=[negA])
    for n in range(16):
        P.op("dve", lambda h, n=n: h.tensor_tensor(out=gg[:, n, :], in0=graw[:, n, 4:8], in1=hps[:, 4:8], op=ALU.add), reads=[graw, hps], writes=[gg])
    P.op("act", lambda h: h.activation(out=gg[:], in_=gg[:], func=AF.Exp), reads=[gg], writes=[gg])
    P.op("dve", lambda h: h.tensor_scalar(out=gg[:], in0=gg[:], scalar1=1.0, scalar2=None, op0=ALU.add), reads=[gg], writes=[gg])
    P.op("act", lambda h: h.activation(out=gg[:], in_=gg[:], func=AF.Ln), reads=[gg], writes=[gg])
    for n in range(16):
        P.op("dve", lambda h, n=n: h.tensor_tensor(out=gg[:, n, :], in0=gg[:, n, :], in1=negA[:], op=ALU.mult), reads=[gg, negA], writes=[gg])
    P.op("pe", lambda h: h.matmul(bank7[:, 0:64], lhsT=Utri[:], rhs=gg[:].rearrange("p n e -> p (n e)"), start=True, stop=True),
         reads=[Utri, gg], writes=[bank7])
    P.op("dve", lambda h: h.tensor_copy(out=gcum[:].rearrange("p n e -> p (n e)"), in_=bank7[:, 0:64]), reads=[bank7], writes=[gcum])
    P.op("act", lambda h: h.activation(out=egc[:], in_=gcum[:], func=AF.Exp), reads=[gcum], writes=[egc])
    P.op("dve", lambda h: h.tensor_tensor(out=beg[:], in0=beta[:], in1=egc[:], op=ALU.mult), reads=[beta, egc], writes=[beg])

    load_w(wtm[0], 512)
    proj_tm(TM1, AF.Silu)
    if stage == "z":
        raise _Stop()

    qT_bf = P.sbuf("qT_bf", [128, S], BF16)
    kT_bf = P.sbuf("kT_bf", [128, S], BF16)
    vT_bf = P.sbuf("vT_bf", [128, S], BF16)
    k_tok = P.sbuf("k_tok", [128, 16, 128], BF16)
    v_tok = P.sbuf("v_tok", [128, 16, 128], BF16)
    o_sb = P.sbuf("o_sb", [128, 16, 128], F32)
    Sst = P.sbuf("Sst", [128, 128], F32)
    S_bf = P.sbuf("S_bf", [128, 128], BF16)
    sm = {}
    for nm in ("diag", "Rsb", "tA", "Ds", "tB", "DiT", "EGR", "L", "LT", "XTa", "XTb", "Pa", "Pb", "PTa", "PTb", "ub", "tmpS"):
        sm[nm] = P.sbuf("g_" + nm, [128, 128], F32)
    smb = {}
    for nm in ("qd", "QKD", "TT", "rv", "rk", "kd", "wT", "u", "pTs", "kit", "SM"):
        smb[nm] = P.sbuf("b_" + nm, [128, 128], BF16)
    kdc = P.sbuf("kdc", [128, 1], F32)
    ssq = P.sbuf("ssq", [128, 16], F32)
    zg = P.sbuf("zg", [128, 128], F32)
    ot = [P.sbuf("ot%d" % i, [128, 128], F32) for i in range(2)]
    evk = [0]

    def evac_copy(dst_ap, dstb, src_ap, srcb):
        k = evk[0]; evk[0] += 1
        if k % 2 == 0:
            P.op("act", lambda h: h.activation(out=dst_ap, in_=src_ap, func=AF.Copy), reads=[srcb], writes=[dstb])
        else:
            P.op("dve", lambda h: h.tensor_copy(out=dst_ap, in_=src_ap), reads=[srcb], writes=[dstb])

    def post_norm(hd_out, gain_lo, gate_buf, hl):
        P.op("dve", lambda h: h.tensor_tensor(out=F1[:, 0:S], in0=o_sb[:].rearrange("p n v -> p (n v)"),
                                              in1=o_sb[:].rearrange("p n v -> p (n v)"), op=ALU.mult), reads=[o_sb], writes=[F1])
        P.op("dve", lambda h: h.tensor_reduce(out=ssq[:], in_=F1[:, 0:S].rearrange("p (n v) -> p n v", v=128), axis=AX.X, op=ALU.add),
             reads=[F1], writes=[ssq])
        P.op("dve", lambda h: h.tensor_scalar(out=ssq[:], in0=ssq[:], scalar1=1.0 / 128, scalar2=RMS_EPS, op0=ALU.mult, op1=ALU.add),
             reads=[ssq], writes=[ssq])
        P.op("act", lambda h: h.activation(out=ssq[:], in_=ssq[:], func=AF.Sqrt), reads=[ssq], writes=[ssq])
        P.op("dve", lambda h: h.reciprocal(out=ssq[:], in_=ssq[:]), reads=[ssq], writes=[ssq])
        for n in range(16):
            o = ot[n % 2]
            P.op("pool", lambda h, n=n: h.tensor_tensor(out=zg[:], in0=gate_buf[:, n, hl * 128:(hl + 1) * 128], in1=nrm[:, gain_lo:gain_lo + 128],
                                                   op=ALU.mult), reads=[gate_buf, nrm], writes=[zg])
            P.op("dve", lambda h, n=n, o=o: h.scalar_tensor_tensor(out=o[:], in0=o_sb[:, n, :], scalar=ssq[:, n:n + 1], in1=zg[:],
                                                                op0=ALU.mult, op1=ALU.mult), reads=[o_sb, ssq, zg], writes=[o])
            P.dma("sp", oout[:, n, hd_out, :], o[:], reads=[o], is_output=True)

    tm2f = TM2.t[:].rearrange("p n f -> p (n f)").bitcast(F32)
    Gs = {1: (F3, F3.t[:, 0:S]), 2: (TM2, tm2f[:, 0:S]), 3: (TM2, tm2f[:, S:2 * S])}
    P.op("dve", lambda h: h.memset(F3[:, 0:4], 0.0), writes=[F3])
    P.op("dve", lambda h: h.memset(tm2f[:, 0:4], 0.0), writes=[TM2])
    P.op("dve", lambda h: h.memset(tm2f[:, S:S + 4], 0.0), writes=[TM2])
    for hl in range(4):
        load_w(wg_fm[hl], 512)
        for j, dstb in enumerate((qT_bf, kT_bf, vT_bf)):
            proj_fm(j * 128, lambda pz, tb: evac_copy(F1[:, tb * 512:(tb + 1) * 512], F1, pz[:], pz))
            for sft in (1, 2, 3):
                Gb, Gap = Gs[sft]
                P.dma("sp" if sft != 2 else "pool", Gap[:, sft:S], F1[:, 0:S - sft], reads=[F1], writes=[Gb])
            cbase = (hl * 3 + j) * 4
            P.op("dve", lambda h, cbase=cbase: h.tensor_scalar(out=F2[:, 0:S], in0=F1[:, 0:S], scalar1=cws[:, cbase + 3:cbase + 4], scalar2=None,
                                                            op0=ALU.mult), reads=[F1, cws], writes=[F2])
            for tap in range(3):
                Gb, Gap = Gs[3 - tap]
                P.op("dve", lambda h, cbase=cbase, tap=tap, Gap=Gap: h.scalar_tensor_tensor(out=F2[:, 0:S], in0=Gap[:, 0:S],
                                                                              scalar=cws[:, cbase + tap:cbase + tap + 1], in1=F2[:, 0:S],
                                                                              op0=ALU.mult, op1=ALU.add), reads=[Gb, cws, F2], writes=[F2])
            if stage in ("c0", "c0x"):
                raise _Stop()
            P.op("act", lambda h: h.activation(out=F2[:, 0:S], in_=F2[:, 0:S], func=AF.Silu), reads=[F2], writes=[F2])
            if stage == "s0":
                raise _Stop()
            if j == 2:
                P.op("act", lambda h: h.activation(out=vT_bf[:], in_=F2[:, 0:S], func=AF.Copy), reads=[F2], writes=[vT_bf])
                continue
            P.op("dve", lambda h: h.tensor_tensor(out=vT_bf[:], in0=F2[:, 0:S], in1=F2[:, 0:S], op=ALU.mult), reads=[F2], writes=[vT_bf])
            for tb in range(4):
                pz = pA if tb % 2 == 0 else pB
                P.op("pe", lambda h, pz=pz, tb=tb: h.matmul(pz[:], lhsT=onesb[:], rhs=vT_bf[:, tb * 512:(tb + 1) * 512], start=True, stop=True),
                     reads=[onesb, vT_bf], writes=[pz])
                P.op("dve", lambda h, pz=pz, tb=tb: h.tensor_scalar(out=F1[:, tb * 512:(tb + 1) * 512], in0=pz[:], scalar1=RMS_EPS, scalar2=None,
                                                              op0=ALU.add), reads=[pz], writes=[F1])
            if stage == "q0":
                raise _Stop()
            P.op("act", lambda h: h.activation(out=F1[:, 0:S], in_=F1[:, 0:S], func=AF.Sqrt), reads=[F1], writes=[F1])
            P.op("dve", lambda h: h.reciprocal(out=F1[:, 0:S], in_=F1[:, 0:S]), reads=[F1], writes=[F1])
            if stage == "r0":
                raise _Stop()
            if j == 0:
                P.op("dve", lambda h: h.scalar_tensor_tensor(out=qT_bf[:], in0=F2[:, 0:S], scalar=SCALE, in1=F1[:, 0:S], op0=ALU.mult, op1=ALU.mult),
                     reads=[F2, F1], writes=[qT_bf])
            else:
                P.op("dve", lambda h: h.tensor_tensor(out=kT_bf[:], in0=F2[:, 0:S], in1=F1[:, 0:S], op=ALU.mult), reads=[F2, F1], writes=[kT_bf])
        if stage == "p1":
            raise _Stop()
        for (srcb, dstb) in ((kT_bf, k_tok), (vT_bf, v_tok)):
            for g0 in range(0, 16, 8):
                for n in range(g0, g0 + 8):
                    P.op("pe", lambda h, n=n, g0=g0, srcb=srcb: h.transpose(out=pT[n - g0][:], in_=srcb[:, n * 128:(n + 1) * 128], identity=idb[:]),
                         reads=[srcb, idb], writes=[bankb])
                P.op("dve", lambda h, g0=g0, dstb=dstb: h.tensor_copy(out=dstb[:, g0:g0 + 8, :].rearrange("p a b -> p (a b)"), in_=bankb[:, 0:1024]),
                     reads=[bankb], writes=[dstb])
        if stage == "t1":
            raise _Stop()
        P.op("dve", lambda h: h.memset(Sst[:], 0.0), writes=[Sst])
        P.op("dve", lambda h: h.memset(S_bf[:], 0.0), writes=[S_bf])
        for n in range(16):
            ck = slice(n * 128, (n + 1) * 128)
            gcol = gcum[:, n, hl:hl + 1]
            P.op("dve", lambda h, gcol=gcol: h.tensor_scalar(out=sm["diag"][:], in0=idf[:], scalar1=gcol, scalar2=None, op0=ALU.mult),
                 reads=[idf, gcum], writes=[sm["diag"]])
            P.op("pe", lambda h: h.matmul(pR[:], lhsT=onesf[:], rhs=sm["diag"][:], start=True, stop=True), reads=[onesf, sm["diag"]], writes=[pR])
            P.op("act", lambda h: h.activation(out=sm["Rsb"][:], in_=pR[:], func=AF.Copy), reads=[pR], writes=[sm["Rsb"]])
            P.op("dve", lambda h, gcol=gcol: h.scalar_tensor_tensor(out=sm["tA"][:], in0=sm["Rsb"][:], scalar=gcol, in1=MBs[:],
                                                                 op0=ALU.subtract, op1=ALU.max), reads=[sm["Rsb"], gcum, MBs], writes=[sm["tA"]])
            P.op("act", lambda h: h.activation(out=sm["Ds"][:], in_=sm["tA"][:], func=AF.Exp, scale=-1.0), reads=[sm["tA"]], writes=[sm["Ds"]])
            P.op("dve", lambda h, gcol=gcol: h.scalar_tensor_tensor(out=sm["tB"][:], in0=sm["Rsb"][:], scalar=gcol, in1=MBi[:],
                                                                 op0=ALU.subtract, op1=ALU.min), reads=[sm["Rsb"], gcum, MBi], writes=[sm["tB"]])
            P.op("act", lambda h: h.activation(out=sm["DiT"][:], in_=sm["tB"][:], func=AF.Exp), reads=[sm["tB"]], writes=[sm["DiT"]])
            P.op("act", lambda h: h.activation(out=sm["EGR"][:], in_=sm["Rsb"][:], func=AF.Exp), reads=[sm["Rsb"]], writes=[sm["EGR"]])
            P.op("dve", lambda h, ck=ck: h.tensor_tensor(out=smb["qd"][:], in0=qT_bf[:, ck], in1=sm["EGR"][:], op=ALU.mult),
                 reads=[qT_bf, sm["EGR"]], writes=[smb["qd"]])
            P.op("dve", lambda h, gcol=gcol: h.tensor_scalar(out=kdc[:], in0=sm["Rsb"][:, 127:128], scalar1=gcol, scalar2=None, op0=ALU.subtract),
                 reads=[sm["Rsb"], gcum], writes=[kdc])
            P.op("act", lambda h: h.activation(out=kdc[:], in_=kdc[:], func=AF.Exp), reads=[kdc], writes=[kdc])
            if stage == "k1":
                raise _Stop()
            P.op("pe", lambda h, ck=ck: h.matmul(pKQ[:, 0:128], lhsT=kT_bf[:, ck], rhs=kT_bf[:, ck], start=True, stop=True), reads=[kT_bf], writes=[pKQ])
            P.op("pe", lambda h, ck=ck: h.matmul(pKQ[:, 128:256], lhsT=kT_bf[:, ck], rhs=qT_bf[:, ck], start=True, stop=True),
                 reads=[kT_bf, qT_bf], writes=[pKQ])
            P.op("dve", lambda h, n=n, hl=hl: h.scalar_tensor_tensor(out=sm["L"][:], in0=pKQ[:, 0:128], scalar=beta[:, n, hl:hl + 1], in1=sm["Ds"][:],
                                                           op0=ALU.mult, op1=ALU.mult), reads=[pKQ, beta, sm["Ds"]], writes=[sm["L"]])
            P.op("dve", lambda h: h.tensor_tensor(out=smb["QKD"][:], in0=pKQ[:, 128:256], in1=sm["DiT"][:], op=ALU.mult),
                 reads=[pKQ, sm["DiT"]], writes=[smb["QKD"]])
            if stage == "k2":
                raise _Stop()
            P.op("pe", lambda h: h.transpose(out=pLT[:], in_=sm["L"][:], identity=idf[:]), reads=[sm["L"], idf], writes=[pLT])
            P.op("act", lambda h: h.activation(out=sm["LT"][:], in_=pLT[:], func=AF.Copy), reads=[pLT], writes=[sm["LT"]])
            P.op("dve", lambda h: h.scalar_tensor_tensor(out=sm["XTa"][:], in0=sm["LT"][:], scalar=-1.0, in1=idf[:], op0=ALU.mult, op1=ALU.add),
                 reads=[sm["LT"], idf], writes=[sm["XTa"]])
            if stage == "k3":
                raise _Stop()
            Pc, PTc, XT = sm["L"], sm["LT"], sm["XTa"]
            pk = 0
            for lev in range(6):
                Pn = sm["Pa"] if lev % 2 == 0 else sm["Pb"]
                PTn = sm["PTa"] if lev % 2 == 0 else sm["PTb"]
                XTn = sm["XTb"] if lev % 2 == 0 else sm["XTa"]
                pz = pN[pk % 4]; pk += 1
                P.op("pe", lambda h, pz=pz, Pc=Pc, PTc=PTc: h.matmul(pz[:], lhsT=PTc[:], rhs=Pc[:], start=True, stop=True), reads=[Pc, PTc], writes=[pz])
                evac_copy(Pn[:], Pn, pz[:], pz)
                if lev < 5:
                    pz2 = pN[pk % 4]; pk += 1
                    P.op("pe", lambda h, pz2=pz2, Pc=Pc, PTc=PTc: h.matmul(pz2[:], lhsT=Pc[:], rhs=PTc[:], start=True, stop=True),
                         reads=[Pc, PTc], writes=[pz2])
                    evac_copy(PTn[:], PTn, pz2[:], pz2)
                pz3 = pN[pk % 4]; pk += 1
                P.op("pe", lambda h, pz3=pz3, Pn=Pn, XT=XT: h.matmul(pz3[:], lhsT=Pn[:], rhs=XT[:], start=True, stop=True), reads=[Pn, XT], writes=[pz3])
                P.op("dve", lambda h, pz3=pz3, XT=XT, XTn=XTn: h.tensor_tensor(out=XTn[:], in0=XT[:], in1=pz3[:], op=ALU.add),
                     reads=[XT, pz3], writes=[XTn])
                Pc, PTc, XT = Pn, PTn, XTn
            P.op("act", lambda h, XT=XT: h.activation(out=smb["TT"][:], in_=XT[:], func=AF.Copy), reads=[XT], writes=[smb["TT"]])
            if stage == "k4":
                raise _Stop()
            P.op("dve", lambda h, n=n, hl=hl: h.tensor_scalar(out=smb["rv"][:], in0=v_tok[:, n, :], scalar1=beta[:, n, hl:hl + 1], scalar2=None, op0=ALU.mult),
                 reads=[v_tok, beta], writes=[smb["rv"]])
            P.op("dve", lambda h, n=n, hl=hl: h.tensor_scalar(out=smb["rk"][:], in0=k_tok[:, n, :], scalar1=beg[:, n, hl:hl + 1], scalar2=None, op0=ALU.mult),
                 reads=[k_tok, beg], writes=[smb["rk"]])
            P.op("dve", lambda h, n=n: h.tensor_scalar(out=smb["kd"][:], in0=k_tok[:, n, :], scalar1=kdc[:, 0:1], scalar2=None, op0=ALU.mult),
                 reads=[k_tok, kdc], writes=[smb["kd"]])
            P.op("pe", lambda h: h.matmul(pU[:], lhsT=smb["TT"][:], rhs=smb["rv"][:], start=True, stop=True), reads=[smb["TT"], smb["rv"]], writes=[pU])
            P.op("act", lambda h: h.activation(out=sm["ub"][:], in_=pU[:], func=AF.Copy), reads=[pU], writes=[sm["ub"]])
            P.op("pe", lambda h: h.matmul(pW[:], lhsT=smb["rk"][:], rhs=smb["TT"][:], start=True, stop=True), reads=[smb["TT"], smb["rk"]], writes=[pW])
            P.op("dve", lambda h: h.tensor_copy(out=smb["wT"][:], in_=pW[:]), reads=[pW], writes=[smb["wT"]])
            if stage == "k5":
                raise _Stop()
            P.op("pe", lambda h: h.matmul(p1[:], lhsT=smb["wT"][:], rhs=S_bf[:], start=True, stop=True), reads=[smb["wT"], S_bf], writes=[p1])
            P.op("dve", lambda h: h.tensor_tensor(out=smb["u"][:], in0=sm["ub"][:], in1=p1[:], op=ALU.subtract), reads=[sm["ub"], p1], writes=[smb["u"]])
            P.op("pe", lambda h: h.matmul(p2[:], lhsT=smb["qd"][:], rhs=S_bf[:], start=True, stop=False), reads=[smb["qd"], S_bf], writes=[p2])
            P.op("pe", lambda h: h.matmul(p2[:], lhsT=smb["QKD"][:], rhs=smb["u"][:], start=False, stop=True), reads=[smb["QKD"], smb["u"]], writes=[p2])
            P.op("act", lambda h, n=n: h.activation(out=o_sb[:, n, :], in_=p2[:], func=AF.Copy), reads=[p2], writes=[o_sb])
            P.op("pe", lambda h: h.matmul(p3[:], lhsT=smb["kd"][:], rhs=smb["u"][:], start=True, stop=True), reads=[smb["kd"], smb["u"]], writes=[p3])
            P.op("dve", lambda h: h.scalar_tensor_tensor(out=Sst[:], in0=Sst[:], scalar=sm["EGR"][:, 127:128], in1=p3[:], op0=ALU.mult, op1=ALU.add),
                 reads=[Sst, sm["EGR"], p3], writes=[Sst])
            P.op("act", lambda h: h.activation(out=S_bf[:], in_=Sst[:], func=AF.Copy), reads=[Sst], writes=[S_bf])
            if stage == "g1":
                raise _Stop()
        post_norm(hl, 0, TM1, hl)
        if stage == "gdn1":
            raise _Stop()

    if stage == "gdn":
        raise _Stop()
    load_w(wtm[1], 512)
    proj_tm(TM1, AF.Copy)
    load_w(wtm[2], 512)
    proj_tm(TM2, AF.Silu)
    lb = P.sbuf("lb", [128, 1], F32)
    oml = P.sbuf("oml", [128, 1], F32)
    l3 = P.sbuf("l3", [128, 3], F32)
    lmx = P.sbuf("lmx", [128, 1], F32)
    lsum = P.sbuf("lsum", [128, 1], F32)
    bmid = P.sbuf("bmid", [128, 16], F32)
    dA = P.sbuf("dA", [128, 16], F32)
    dM = P.sbuf("dM", [128, 16], F32)
    dB = P.sbuf("dB", [128, 16], F32)
    qdT = qT_bf; kiT = kT_bf
    for hl in range(4):
        P.op("dve", lambda h, hl=hl: h.tensor_reduce(out=lmx[:], in_=lbs[:, hl * 3:hl * 3 + 3], axis=AX.X, op=ALU.max), reads=[lbs], writes=[lmx])
        P.op("dve", lambda h: h.tensor_scalar(out=lmx[:], in0=lmx[:], scalar1=-1.0, scalar2=None, op0=ALU.mult), reads=[lmx], writes=[lmx])
        P.op("act", lambda h, hl=hl: h.activation(out=l3[:], in_=lbs[:, hl * 3:hl * 3 + 3], func=AF.Exp, bias=lmx[:, 0:1], scale=1.0),
             reads=[lbs, lmx], writes=[l3])
        P.op("dve", lambda h: h.tensor_reduce(out=lsum[:], in_=l3[:], axis=AX.X, op=ALU.add), reads=[l3], writes=[lsum])
        P.op("dve", lambda h: h.reciprocal(out=lsum[:], in_=lsum[:]), reads=[lsum], writes=[lsum])
        P.op("dve", lambda h: h.tensor_tensor(out=lb[:], in0=l3[:, 0:1], in1=lsum[:], op=ALU.mult), reads=[l3, lsum], writes=[lb])
        P.op("dve", lambda h: h.tensor_scalar(out=oml[:], in0=lb[:], scalar1=-1.0, scalar2=1.0, op0=ALU.mult, op1=ALU.add), reads=[lb], writes=[oml])
        load_w(wh_fm[hl], 512)
        proj_fm(128, lambda pz, tb: P.op("act", lambda h, pz=pz, tb=tb: h.activation(out=F2[:, tb * 512:(tb + 1) * 512], in_=pz[:], func=AF.Sigmoid),
                                         reads=[pz], writes=[F2]))
        P.op("dve", lambda h: h.tensor_scalar(out=F2[:, 0:S], in0=F2[:, 0:S], scalar1=oml[:, 0:1], scalar2=lb[:, 0:1], op0=ALU.mult, op1=ALU.add),
             reads=[F2, oml, lb], writes=[F2])
        P.op("act", lambda h: h.activation(out=F1[:, 0:S], in_=F2[:, 0:S], func=AF.Ln), reads=[F2], writes=[F1])
        P.op("dve", lambda h: h.tensor_tensor_scan(out=F3[:, 0:S], data0=rm[:], data1=F1[:, 0:S], initial=0.0, op0=ALU.mult, op1=ALU.add),
             reads=[rm, F1], writes=[F3])
        b3 = F3[:, 0:S].rearrange("p (n t) -> p n t", t=128)
        P.op("dve", lambda h: h.tensor_copy(out=bmid[:], in_=b3[:, :, 63]), reads=[F3], writes=[bmid])
        P.op("act", lambda h: h.activation(out=dA[:], in_=b3[:, :, 127], func=AF.Exp), reads=[F3], writes=[dA])
        P.op("act", lambda h: h.activation(out=dM[:], in_=bmid[:], func=AF.Exp), reads=[bmid], writes=[dM])
        P.op("dve", lambda h: h.tensor_tensor(out=dB[:], in0=b3[:, :, 127], in1=bmid[:], op=ALU.subtract), reads=[F3, bmid], writes=[dB])
        P.op("act", lambda h: h.activation(out=dB[:], in_=dB[:], func=AF.Exp), reads=[dB], writes=[dB])
        for n in range(16):
            P.op("dve", lambda h, n=n: h.tensor_scalar(out=F3[:, n * 128:(n + 1) * 128], in0=F3[:, n * 128:(n + 1) * 128], scalar1=bmid[:, n:n + 1],
                                                     scalar2=None, op0=ALU.subtract), reads=[F3, bmid], writes=[F3])
        P.op("dve", lambda h: h.tensor_scalar(out=F2[:, 0:S], in0=F2[:, 0:S], scalar1=-1.0, scalar2=1.0, op0=ALU.mult, op1=ALU.add), reads=[F2], writes=[F2])
        P.op("act", lambda h: h.activation(out=F1[:, 0:S], in_=F3[:, 0:S], func=AF.Exp, scale=-1.0), reads=[F3], writes=[F1])
        P.op("dve", lambda h: h.tensor_tensor(out=kiT[:], in0=F2[:, 0:S], in1=F1[:, 0:S], op=ALU.mult), reads=[F2, F1], writes=[kiT])
        P.op("act", lambda h: h.activation(out=F3[:, 0:S], in_=F3[:, 0:S], func=AF.Exp), reads=[F3], writes=[F3])
        proj_fm(0, lambda pz, tb: P.op("dve", lambda h, pz=pz, tb=tb: h.tensor_tensor(out=qdT[:, tb * 512:(tb + 1) * 512], in0=pz[:],
                                                                                 in1=F3[:, tb * 512:(tb + 1) * 512], op=ALU.mult),
                                       reads=[pz, F3], writes=[qdT]))
        P.op("dve", lambda h: h.memset(Sst[:], 0.0), writes=[Sst])
        for n in range(16):
            ck = slice(n * 128, (n + 1) * 128)
            iv = TM1[:, n, hl * 128:(hl + 1) * 128]
            P.op("pe", lambda h, ck=ck: h.matmul(hPp[:], lhsT=kiT[:, ck], rhs=qdT[:, ck], start=True, stop=True), reads=[kiT, qdT], writes=[hPp])
            P.op("dve", lambda h: h.scalar_tensor_tensor(out=smb["pTs"][:], in0=hPp[:], scalar=3e38, in1=Utri[:], op0=ALU.min, op1=ALU.mult),
                 reads=[hPp, Utri], writes=[smb["pTs"]])
            P.op("dve", lambda h, n=n: h.tensor_scalar(out=smb["SM"][:], in0=Sst[:], scalar1=dM[:, n:n + 1], scalar2=None, op0=ALU.mult),
                 reads=[Sst, dM], writes=[smb["SM"]])
            P.op("pe", lambda h, ck=ck: h.matmul(hOp[:], lhsT=qdT[:, ck], rhs=smb["SM"][:], start=True, stop=False), reads=[qdT, smb["SM"]], writes=[hOp])
            P.op("pe", lambda h, iv=iv: h.matmul(hOp[:], lhsT=smb["pTs"][:], rhs=iv, start=False, stop=True), reads=[smb["pTs"], TM1], writes=[hOp])
            P.op("act", lambda h, n=n: h.activation(out=o_sb[:, n, :], in_=hOp[:], func=AF.Copy), reads=[hOp], writes=[o_sb])
            P.op("pe", lambda h, ck=ck: h.transpose(out=pT[0][:], in_=kiT[:, ck], identity=idb[:]), reads=[kiT, idb], writes=[pT[0]])
            P.op("dve", lambda h: h.tensor_copy(out=smb["kit"][:], in_=pT[0][:]), reads=[pT[0]], writes=[smb["kit"]])
            P.op("pe", lambda h, iv=iv: h.matmul(hSp[:], lhsT=smb["kit"][:], rhs=iv, start=True, stop=True), reads=[smb["kit"], TM1], writes=[hSp])
            P.op("dve", lambda h, n=n: h.tensor_scalar(out=sm["tmpS"][:], in0=hSp[:], scalar1=dB[:, n:n + 1], scalar2=None, op0=ALU.mult),
                 reads=[hSp, dB], writes=[sm["tmpS"]])
            P.op("dve", lambda h, n=n: h.scalar_tensor_tensor(out=Sst[:], in0=Sst[:], scalar=dA[:, n:n + 1], in1=sm["tmpS"][:], op0=ALU.mult, op1=ALU.add),
                 reads=[Sst, dA, sm["tmpS"]], writes=[Sst])
        post_norm(4 + hl, 128, TM2, hl)


def a0_consts():
    i = np.arange(128)[:, None]; j = np.arange(128)[None, :]
    ident = np.eye(128); ones = np.ones((128, 128))
    MBs = np.where(j < i, 0.0, 1e30)
    MBi = np.where(j >= i, 0.0, NEG)
    Utri = np.where(i <= j, 1.0, 0.0)
    cm = np.stack([ident, ones, MBs, MBi, Utri], 1).astype(np.float32)
    tri_u = np.where(i <= j, 1, 0).astype(np.uint32)
    rmask = np.ones((128, 2048), np.float32); rmask[:, ::128] = 0.0
    return np.ascontiguousarray(cm), tri_u, rmask


A0_STAGE = "full"


def run_a0(x, w_in, conv_w, a_log, dt_bias, gdn_norm, hgrn_norm, lb_logits):
    nc = _get("a0", build_a0, A0_STAGE)
    cm, tri_u, rmask = a0_consts()

    def lay(wc):
        return wc.reshape(16, 128, -1).transpose(1, 0, 2)
    maps = []
    for core in range(8):
        b, half = core // 2, core % 2
        hT = np.ascontiguousarray(x[b].T.reshape(16, 128, 2048).transpose(1, 0, 2))
        heads = [half * 4 + i for i in range(4)]
        wg_fm = np.stack([lay(np.concatenate([w_in[:, h * 128:(h + 1) * 128], w_in[:, 1024 + h * 128:1024 + (h + 1) * 128],
                                              w_in[:, 2048 + h * 128:2048 + (h + 1) * 128], w_in[:, 0:128]], 1)) for h in heads], 0)
        wh_fm = np.stack([lay(np.concatenate([w_in[:, 4112 + h * 128:4112 + (h + 1) * 128], w_in[:, 5136 + h * 128:5136 + (h + 1) * 128],
                                              w_in[:, 0:256]], 1)) for h in heads], 0)
        h0 = heads[0] * 128
        wtm = np.stack([lay(w_in[:, 3072 + h0:3072 + h0 + 512]), lay(w_in[:, 6160 + h0:6160 + h0 + 512]), lay(w_in[:, 7184 + h0:7184 + h0 + 512])], 0)
        wgate = lay(np.concatenate([w_in[:, 4096 + heads[0]:4096 + heads[0] + 4], w_in[:, 4104 + heads[0]:4104 + heads[0] + 4]], 1))
        cwl = np.empty((128, 4, 3, 4), np.float32)
        for i, h in enumerate(heads):
            for j in range(3):
                cwl[:, i, j, :] = conv_w[:, j * 1024 + h * 128:j * 1024 + (h + 1) * 128].T
        hp = bc(np.concatenate([a_log[heads[0]:heads[0] + 4], dt_bias[heads[0]:heads[0] + 4]]))
        norms = bc(np.concatenate([gdn_norm, hgrn_norm]))
        lbl = np.stack([lb_logits[:, h * 128:(h + 1) * 128].T for h in heads], 1)
        maps.append({"hT": hT, "wg_fm": np.ascontiguousarray(wg_fm), "wh_fm": np.ascontiguousarray(wh_fm), "wtm": np.ascontiguousarray(wtm),
                     "wgate": np.ascontiguousarray(wgate), "cw": cwl, "hp": hp, "norms": norms, "lbl": np.ascontiguousarray(lbl),
                     "cm": cm, "rmask": rmask})
    res = run_bass_kernel_spmd(nc, maps, core_ids=list(range(8)))
    o = np.empty((4, 2048, 16, 128), np.float32)
    for core in range(8):
        b, half = core // 2, core % 2
        r = res.results[core]["oout"].transpose(1, 0, 2, 3).reshape(2048, 8, 128)
        o[b, :, half * 4:half * 4 + 4] = r[:, 0:4]
        o[b, :, 8 + half * 4:8 + half * 4 + 4] = r[:, 4:8]
    return o.reshape(4, 2048, 2048)


def kernel(x, ev_w_in, ev_conv_w, ev_a_log, ev_dt_bias, ev_gdn_norm, ev_hgrn_norm, hgrn_lb_logits,
           ev_w_out, od_w_in, od_w_out, router_w, router_bias, moe_w_gate, moe_w_up, moe_w_down,
           ln_gain, ln_bias):
    f = lambda a: np.asarray(a, np.float32)
    x = f(x)
    B, S, Dm = x.shape
    o0 = run_a0(x, f(ev_w_in)[0], f(ev_conv_w)[0], f(ev_a_log)[0], f(ev_dt_bias)[0], f(ev_gdn_norm)[0], f(ev_hgrn_norm)[0],
                f(hgrn_lb_logits))
    h = run_k1(o0.reshape(B * S, -1), x.reshape(B * S, Dm), f(ev_w_out)[0], f(ln_gain)[0, 0], f(ln_bias)[0, 0])
    h = run_k2(h, f(router_w), f(router_bias), f(moe_w_gate)[0], f(moe_w_up)[0], f(moe_w_down)[0], f(ln_gain)[0, 1], f(ln_bias)[0, 1])
    o1 = run_a1(h.reshape(B, S, Dm), f(od_w_in)[0])
    h = run_k1(o1.reshape(B * S, -1), h, f(od_w_out)[0], f(ln_gain)[1, 0], f(ln_bias)[1, 0])
    h = run_k2(h, f(router_w), f(router_bias), f(moe_w_gate)[1], f(moe_w_up)[1], f(moe_w_down)[1], f(ln_gain)[1, 1], f(ln_bias)[1, 1])
    return h.reshape(B, S, Dm).astype(np.float32)
```
